# Optimizing a Trainium2 kernel written in Bass

```python
import math
import jax, jax.numpy as jnp
from jax import lax
import numpy as np

D_MODEL = 2048
BATCH = 1
SEQ = 8192
DEPTH = 1

HEAD_DIM = 128
A_HEADS = 8
A_KV_HEADS = 2
B_HEADS = 8
B_KV_HEADS = 2
WINDOW = 128
BLOCK = 128
N_BUCKETS = 32
MAX_DISTANCE = 128
GRID_W = 64
ROPE_THETA = 10000.0
N_EXPERTS = 64
TOP_K = 8
N_GROUPS = 8
TOPK_GROUPS = 4
EXPERT_HIDDEN = 512
SHARED_HIDDEN = 512
ROUTED_SCALE = 2.5
MOE_BLOCK = 128
LN_EPS = 1e-5
RMS_EPS = 1e-6
DEEPNORM_ALPHA = (2 * DEPTH) ** 0.25
DEEPNORM_BETA = (8 * DEPTH) ** -0.25

A_Q = A_HEADS * HEAD_DIM
A_KV = A_KV_HEADS * HEAD_DIM
B_Q = B_HEADS * HEAD_DIM
B_KV = B_KV_HEADS * HEAD_DIM
IN_COLS = A_Q + 2 * A_KV + B_Q + 2 * B_KV + 2 * D_MODEL

kernel_name = "hybrid_window_axial_moe_encoder"


def layer_norm(x, g, b):
    xf = x.astype(jnp.float32)
    mu = jnp.mean(xf, -1, keepdims=True)
    var = jnp.mean(jnp.square(xf - mu), -1, keepdims=True)
    return ((xf - mu) * lax.rsqrt(var + LN_EPS) * g.astype(jnp.float32) + b.astype(jnp.float32)).astype(x.dtype)


def rms_norm(x, g):
    xf = x.astype(jnp.float32)
    y = xf * lax.rsqrt(jnp.mean(jnp.square(xf), -1, keepdims=True) + RMS_EPS)
    return (y * g.astype(jnp.float32)).astype(x.dtype)


def t5_bucket(rel):
    nb = N_BUCKETS // 2
    ret = jnp.where(rel > 0, nb, 0)
    n = jnp.abs(rel)
    max_exact = nb // 2
    nf = jnp.maximum(n, 1).astype(jnp.float32)
    large = max_exact + (jnp.log(nf / max_exact) / math.log(MAX_DISTANCE / max_exact) * (nb - max_exact)).astype(jnp.int32)
    large = jnp.minimum(large, nb - 1)
    return ret + jnp.where(n < max_exact, n, large)


def windowed_sink_attention(q, k, v, sink, rel_table):
    Bsz, S = q.shape[0], q.shape[1]
    nblk = S // BLOCK
    G = A_HEADS // A_KV_HEADS
    qb = q.reshape(Bsz, nblk, BLOCK, A_KV_HEADS, G, HEAD_DIM)
    pad = ((0, 0), (BLOCK, BLOCK), (0, 0))

    def band(t):
        tp = jnp.pad(t, pad)
        return jnp.concatenate(
            [tp[:, j * BLOCK:(j + nblk) * BLOCK].reshape(Bsz, nblk, BLOCK, A_KV_HEADS, HEAD_DIM) for j in range(3)],
            axis=2)

    kb, vb = band(k), band(v)
    s = jnp.einsum('bnqkgd,bnckd->bnkgqc', qb, kb, preferred_element_type=jnp.float32) * (HEAD_DIM ** -0.5)
    qi = jnp.arange(BLOCK)[:, None]
    c = jnp.arange(3 * BLOCK)[None, :]
    rel = c - BLOCK - qi
    bias = rel_table.astype(jnp.float32)[t5_bucket(rel)]
    bias = jnp.moveaxis(bias, -1, 0).reshape(A_KV_HEADS, G, BLOCK, 3 * BLOCK)
    kpos = (jnp.arange(nblk)[:, None] - 1) * BLOCK + jnp.arange(3 * BLOCK)[None, :]
    valid = (jnp.abs(rel) <= WINDOW)[None] & ((kpos >= 0) & (kpos < S))[:, None, :]
    s = jnp.where(valid[None, :, None, None], s + bias, -jnp.inf)
    sk = sink.astype(jnp.float32).reshape(A_KV_HEADS, G)[:, :, None, None]
    m = jnp.maximum(jnp.max(s, -1, keepdims=True), sk)
    p = jnp.exp(s - m)
    p = p / (jnp.sum(p, -1, keepdims=True) + jnp.exp(sk - m))
    o = jnp.einsum('bnkgqc,bnckd->bnqkgd', p.astype(v.dtype), vb)
    return o.reshape(Bsz, S, A_Q)


def axial_rope(x, cos_r, sin_r, cos_c, sin_c):
    half = HEAD_DIM // 2
    qd = half // 2

    def rot(xs, cs, sn):
        x1, x2 = xs[..., :qd], xs[..., qd:]
        return jnp.concatenate([x1 * cs - x2 * sn, x2 * cs + x1 * sn], -1)

    return jnp.concatenate([rot(x[..., :half], cos_r, sin_r), rot(x[..., half:], cos_c, sin_c)], -1)


def axial_dense_attention(q, k, v, qn_g, kn_g):
    Bsz, S = q.shape[0], q.shape[1]
    ROWS = S // GRID_W
    nblk = S // BLOCK
    G = B_HEADS // B_KV_HEADS
    row = jnp.broadcast_to(jnp.arange(ROWS)[:, None], (ROWS, GRID_W)).reshape(S).astype(jnp.float32)
    col = jnp.broadcast_to(jnp.arange(GRID_W)[None, :], (ROWS, GRID_W)).reshape(S).astype(jnp.float32)
    half = HEAD_DIM // 2
    inv = ROPE_THETA ** (-jnp.arange(0, half, 2, dtype=jnp.float32) / half)
    ang_r = (row[:, None] * inv)[:, None, :]
    ang_c = (col[:, None] * inv)[:, None, :]
    dt = q.dtype
    tabs = (jnp.cos(ang_r).astype(dt), jnp.sin(ang_r).astype(dt), jnp.cos(ang_c).astype(dt), jnp.sin(ang_c).astype(dt))
    qh = axial_rope(rms_norm(q.reshape(Bsz, S, B_HEADS, HEAD_DIM), qn_g), *tabs)
    kh = axial_rope(rms_norm(k.reshape(Bsz, S, B_KV_HEADS, HEAD_DIM), kn_g), *tabs)
    vh = v.reshape(Bsz, S, B_KV_HEADS, HEAD_DIM)
    qb = qh.reshape(Bsz, nblk, BLOCK, B_KV_HEADS, G, HEAD_DIM).transpose(1, 0, 2, 3, 4, 5)
    scale = HEAD_DIM ** -0.5

    def one_block(qblk):
        s = jnp.einsum('bqkgd,bskd->bkgqs', qblk, kh, preferred_element_type=jnp.float32) * scale
        p = jax.nn.softmax(s, axis=-1)
        return jnp.einsum('bkgqs,bskd->bqkgd', p.astype(vh.dtype), vh)

    o = lax.map(one_block, qb)
    return o.transpose(1, 0, 2, 3, 4, 5).reshape(Bsz, S, B_Q)


def token_mixer(h, w_in, b_gate, sink, rel_table, qn_g, kn_g, w_branch_a, w_branch_b, w_out):
    proj = jnp.einsum('bsd,dc->bsc', h, w_in)
    bounds = [int(i) for i in np.cumsum([A_Q, A_KV, A_KV, B_Q, B_KV, B_KV, D_MODEL])]
    qa, ka, va, qb, kb, vb, ga, gb = jnp.split(proj, bounds, axis=-1)
    oa = windowed_sink_attention(qa, ka, va, sink, rel_table)
    ob = axial_dense_attention(qb, kb, vb, qn_g, kn_g)
    gate_a = jax.nn.sigmoid(ga + b_gate[0])
    gate_b = jax.nn.sigmoid(gb + b_gate[1])
    merged = gate_a * jnp.einsum('bsc,cd->bsd', oa, w_branch_a) + gate_b * jnp.einsum('bsc,cd->bsd', ob, w_branch_b)
    return jnp.einsum('bsd,de->bse', merged, w_out)


def moe_ffn(h, w_router, e_bias, w_gate, w_up, w_down, ws_gate, ws_up, ws_down):
    Bsz, S, D = h.shape
    N = Bsz * S
    xt = h.reshape(N, D)
    scores = jax.nn.sigmoid(jnp.einsum('nd,de->ne', xt, w_router, preferred_element_type=jnp.float32))
    biased = scores + e_bias.astype(jnp.float32)
    group_score = lax.top_k(biased.reshape(N, N_GROUPS, N_EXPERTS // N_GROUPS), 2)[0].sum(-1)
    _, gidx = lax.top_k(group_score, TOPK_GROUPS)
    gmask = jnp.sum(jax.nn.one_hot(gidx, N_GROUPS, dtype=jnp.float32), axis=1) > 0
    masked = jnp.where(jnp.repeat(gmask, N_EXPERTS // N_GROUPS, axis=1), biased, -jnp.inf)
    _, eidx = lax.top_k(masked, TOP_K)
    wsel = jnp.take_along_axis(scores, eidx, axis=1)
    wsel = wsel / jnp.sum(wsel, -1, keepdims=True) * ROUTED_SCALE

    A = N * TOP_K
    e_flat = eidx.reshape(A)
    tok_flat = jnp.arange(A, dtype=jnp.int32) // TOP_K
    w_flat = wsel.reshape(A)
    order = jnp.argsort(e_flat)
    e_sorted = e_flat[order]
    counts = jnp.bincount(e_flat, length=N_EXPERTS)
    starts = jnp.cumsum(counts) - counts
    padded = (counts + MOE_BLOCK - 1) // MOE_BLOCK * MOE_BLOCK
    pends = jnp.cumsum(padded)
    pstarts = pends - padded
    dest = pstarts[e_sorted] + jnp.arange(A, dtype=jnp.int32) - starts[e_sorted]
    nb = -(-A // MOE_BLOCK) + N_EXPERTS
    P = nb * MOE_BLOCK
    tok_buf = jnp.full((P,), N, jnp.int32).at[dest].set(tok_flat[order])
    w_buf = jnp.zeros((P,), jnp.float32).at[dest].set(w_flat[order])
    block_e = jnp.minimum(jnp.searchsorted(pends, jnp.arange(nb) * MOE_BLOCK, side='right'), N_EXPERTS - 1)
    x_pad = jnp.concatenate([xt, jnp.zeros((1, D), xt.dtype)], axis=0)

    def run_block(args):
        e, toks, wts = args
        xb = x_pad[toks]
        hid = jax.nn.silu(xb @ w_gate[e]) * (xb @ w_up[e])
        return (hid @ w_down[e]) * wts[:, None].astype(xb.dtype)

    yb = lax.map(run_block, (block_e, tok_buf.reshape(nb, MOE_BLOCK), w_buf.reshape(nb, MOE_BLOCK)))
    routed = jnp.zeros((N + 1, D), jnp.float32).at[tok_buf].add(yb.reshape(P, D).astype(jnp.float32))[:N]
    shared = (jax.nn.silu(xt @ ws_gate) * (xt @ ws_up)) @ ws_down
    return (routed.astype(h.dtype) + shared).reshape(Bsz, S, D)


def setup_inputs(seed: int = 0) -> dict:
    key = jax.random.key(seed)
    ks = jax.random.split(key, 24)
    f32 = jnp.float32
    L, D, E, F, Fs = DEPTH, D_MODEL, N_EXPERTS, EXPERT_HIDDEN, SHARED_HIDDEN

    def nrm(k, shape, scale):
        return jax.random.normal(k, shape, f32) * scale

    colscale = np.concatenate([np.ones(A_Q), np.ones(A_KV), np.full(A_KV, DEEPNORM_BETA), np.ones(B_Q), np.ones(B_KV),
                               np.full(B_KV, DEEPNORM_BETA), np.ones(2 * D)]).astype(np.float32)
    return {
        "x": nrm(ks[0], (BATCH, SEQ, D), 1.0),
        "w_in": nrm(ks[1], (L, D, IN_COLS), D ** -0.5) * jnp.asarray(colscale),
        "b_gate": nrm(ks[2], (L, 2, D), 0.02),
        "attn_sink": nrm(ks[3], (L, A_HEADS), 0.5),
        "rel_bias_table": nrm(ks[4], (N_BUCKETS, A_HEADS), 0.5),
        "q_norm_g": 1.0 + nrm(ks[5], (L, HEAD_DIM), 0.02),
        "k_norm_g": 1.0 + nrm(ks[6], (L, HEAD_DIM), 0.02),
        "w_branch_a": nrm(ks[7], (L, A_Q, D), A_Q ** -0.5),
        "w_branch_b": nrm(ks[8], (L, B_Q, D), B_Q ** -0.5),
        "w_out": nrm(ks[9], (L, D, D), D ** -0.5 * DEEPNORM_BETA),
        "ln1_g": 1.0 + nrm(ks[10], (L, D), 0.02),
        "ln1_b": nrm(ks[11], (L, D), 0.02),
        "w_router": nrm(ks[12], (L, D, E), D ** -0.5),
        "router_bias": nrm(ks[13], (L, E), 0.01),
        "w_exp_gate": nrm(ks[14], (L, E, D, F), D ** -0.5),
        "w_exp_up": nrm(ks[15], (L, E, D, F), D ** -0.5),
        "w_exp_down": nrm(ks[16], (L, E, F, D), F ** -0.5 * DEEPNORM_BETA),
        "w_sh_gate": nrm(ks[17], (L, D, Fs), D ** -0.5),
        "w_sh_up": nrm(ks[18], (L, D, Fs), D ** -0.5),
        "w_sh_down": nrm(ks[19], (L, Fs, D), Fs ** -0.5 * DEEPNORM_BETA),
        "ln2_g": 1.0 + nrm(ks[20], (L, D), 0.02),
        "ln2_b": nrm(ks[21], (L, D), 0.02),
    }


def reference(x, w_in, b_gate, attn_sink, rel_bias_table, q_norm_g, k_norm_g, w_branch_a, w_branch_b, w_out,
              ln1_g, ln1_b, w_router, router_bias, w_exp_gate, w_exp_up, w_exp_down, w_sh_gate, w_sh_up, w_sh_down,
              ln2_g, ln2_b):
    h = x
    for l in range(DEPTH):
        mix = token_mixer(h, w_in[l], b_gate[l], attn_sink[l], rel_bias_table, q_norm_g[l], k_norm_g[l],
                          w_branch_a[l], w_branch_b[l], w_out[l])
        h = layer_norm(DEEPNORM_ALPHA * h + mix, ln1_g[l], ln1_b[l])
        ffn = moe_ffn(h, w_router[l], router_bias[l], w_exp_gate[l], w_exp_up[l], w_exp_down[l],
                      w_sh_gate[l], w_sh_up[l], w_sh_down[l])
        h = layer_norm(DEEPNORM_ALPHA * h + ffn, ln2_g[l], ln2_b[l])
    return h
```

```python
import math
from contextlib import ExitStack

import numpy as np
import concourse.bass as bass
import concourse.mybir as mybir
from concourse.bass_utils import run_bass_kernel_spmd

F32 = mybir.dt.float32
BF16 = mybir.dt.bfloat16
I32 = mybir.dt.int32
U8 = mybir.dt.uint8
AF = mybir.ActivationFunctionType
ALU = mybir.AluOpType
AX = mybir.AxisListType

NCORES = 8
S = 8192
D = 2048
TOK = S // NCORES
NT = TOK // 128
HALO = 128
TH = TOK + 2 * HALO
DH = 128
NH = 8
NKV = 2
E = 64
FF = 512
CAP = 256
IN_COLS = 7168
C_QA, C_KA, C_VA, C_QB, C_KB, C_VB, C_GA, C_GB = 0, 1024, 1280, 1536, 2560, 2816, 3072, 5120
ALPHA = 2.0 ** 0.25
SCALE = DH ** -0.5
NEG = -30000.0
BIG = 1.0e9

NDSEM = 8


class Op:
    __slots__ = ("eng", "fn", "dma", "deps", "sig", "sval", "dsem", "dval")

    def __init__(self, eng, fn, dma):
        self.eng = eng
        self.fn = fn
        self.dma = dma
        self.deps = ()
        self.sig = False
        self.sval = 0
        self.dsem = None
        self.dval = 0


class Prog:
    ENGS = ("pe", "act", "dve", "pool", "sp")

    def __init__(self, nc, stack):
        self.nc = nc
        self.stack = stack
        self.ops = []
        self.last_w = {}
        self.readers = {}
        self.last_c = {}
        self.dq = {"sp": [], "pool": [], "act": []}
        self.regs = {}

    def op(self, eng, fn, reads=(), writes=(), dma=False, extra=(), pwrites=()):
        idx = len(self.ops)
        o = Op(eng, fn, dma)
        deps = set(extra)
        for r in reads:
            deps.update(self.last_w.get(r, ()))
        for w in writes:
            deps.update(self.last_w.get(w, ()))
            rd = self.readers.get(w)
            if rd:
                deps.update(rd[0].values())
                deps.update(rd[1])
        for w in pwrites:
            rd = self.readers.get(w)
            if rd and (rd[0] or rd[1]):
                deps.update(rd[0].values())
                deps.update(rd[1])
                self.last_w[w] = []
                self.readers[w] = ({}, [])
        for r in reads:
            rd = self.readers.get(r)
            if rd is None:
                rd = self.readers[r] = ({}, [])
            if dma:
                rd[1].append(idx)
            else:
                rd[0][eng] = idx
        for w in writes:
            self.last_w[w] = [idx]
            self.readers[w] = ({}, [])
        for w in pwrites:
            self.last_w.setdefault(w, []).append(idx)
        deps.discard(idx)
        o.deps = tuple(deps)
        self.ops.append(o)
        if dma:
            self.dq[eng].append(idx)
        elif fn is not None:
            self.last_c[eng] = idx
        return idx

    def dma(self, eng, out, in_, reads=(), writes=(), pwrites=(), **kw):
        return self.op(eng, lambda e: e.dma_start(out=out, in_=in_, **kw), reads, writes, dma=True, pwrites=pwrites)

    def reg(self, e, val):
        if val not in self.regs:
            self.regs[val] = e.to_reg(val)
        return self.regs[val]

    def barrier(self):
        deps = set(self.last_c.values())
        for q in self.dq.values():
            deps.update(q[-NDSEM:])
        for e in self.ENGS:
            self.op(e, None, extra=tuple(deps))

    def finalize(self):
        nc = self.nc
        ops = self.ops
        for o in ops:
            for d in o.deps:
                p = ops[d]
                if p.eng == "pe" and o.eng == "pe" and not p.dma and not o.dma:
                    continue
                p.sig = True
        csem = {e: self.stack.enter_context(nc.semaphore(f"c_{e}")) for e in self.ENGS}
        dsems = {e: [self.stack.enter_context(nc.semaphore(f"d_{e}{i}")) for i in range(NDSEM)]
                 for e in ("sp", "pool", "act")}
        cnt = {e: 0 for e in self.ENGS}
        dcnt = {e: 0 for e in dsems}
        for o in ops:
            if o.dma:
                j = dcnt[o.eng]
                dcnt[o.eng] += 1
                o.dsem = (o.eng, j % NDSEM)
                o.dval = 16 * (j // NDSEM + 1)
            elif o.sig:
                assert o.fn is not None
                cnt[o.eng] += 1
                o.sval = cnt[o.eng]
        self.stats = dict(cnt=cnt, dcnt=dcnt, nops=len(ops))

        def run(engname, e):
            waited = {}
            for o in ops:
                if o.eng != engname:
                    continue
                need = {}
                for d in o.deps:
                    p = ops[d]
                    if p.dma:
                        key = ("d",) + p.dsem
                        val = p.dval
                    else:
                        if p.eng == "pe" and engname == "pe" and not o.dma:
                            continue
                        key = ("c", p.eng)
                        val = p.sval
                    if val > need.get(key, 0):
                        need[key] = val
                if o.dma and o.dval > 16:
                    key = ("d",) + o.dsem
                    need[key] = max(need.get(key, 0), o.dval - 16)
                for key, val in need.items():
                    if val > waited.get(key, 0):
                        sem = csem[key[1]] if key[0] == "c" else dsems[key[1]][key[2]]
                        e.wait_ge(sem, val)
                        waited[key] = val
                if o.fn is None:
                    continue
                ins = o.fn(e)
                if o.dma:
                    ins.then_inc(dsems[o.dsem[0]][o.dsem[1]], 16)
                elif o.sig:
                    ins.then_inc(csem[engname], 1)

        with nc.Block() as block:
            @block.tensor
            def _(e):
                run("pe", e)

            @block.scalar
            def _(e):
                run("act", e)

            @block.vector
            def _(e):
                run("dve", e)

            @block.gpsimd
            def _(e):
                run("pool", e)

            @block.sync
            def _(e):
                run("sp", e)


_DTSZ = {F32: 4, BF16: 2, I32: 4, U8: 1}


class Arena:
    def __init__(self, raw, nbytes):
        self.raw = raw
        self.free = [(0, nbytes)]
        self.live = {}
        self.peak = 0

    def alloc(self, name, shape, dt):
        n = 1
        for s in shape[1:]:
            n *= s
        nb = (n * _DTSZ[dt] + 63) // 64 * 64
        for i, (off, sz) in enumerate(self.free):
            if sz >= nb:
                if sz == nb:
                    self.free.pop(i)
                else:
                    self.free[i] = (off + nb, sz - nb)
                break
        else:
            raise MemoryError(f"arena: cannot alloc {name} {nb}B; free={self.free}")
        self.live[name] = (off, nb)
        self.peak = max(self.peak, off + nb)
        ap = self.raw[:, off:off + n * _DTSZ[dt]].bitcast(dt)
        if len(shape) == 3:
            ap = ap.rearrange("p (a b) -> p a b", a=shape[1])
        elif len(shape) == 4:
            ap = ap.rearrange("p (a b c) -> p a b c", a=shape[1], b=shape[2])
        return ap

    def release(self, *names):
        for name in names:
            off, nb = self.live.pop(name)
            self.free.append((off, nb))
        self.free.sort()
        merged = []
        for off, sz in self.free:
            if merged and merged[-1][0] + merged[-1][1] == off:
                merged[-1] = (merged[-1][0], merged[-1][1] + sz)
            else:
                merged.append((off, sz))
        self.free = merged


CP_IDENT, CP_IOTA, CP_U, CP_IDXA, CP_MASKA, CP_VCOL, CP_SLOT, CP_N = 0, 128, 384, 512, 896, 1280, 1288, 1290
PP_SINK, PP_TAB, PP_GQ, PP_GK, PP_RB, PP_BG, PP_EDGE, PP_N = 0, 8, 264, 392, 520, 584, 616, 618


class K:
    pass


def build_program(stop_after=99, dbg=False):
    nc = bass.Bass("TRN2", target_bir_lowering=False)
    k = K()
    k.nc = nc
    k.dbg = dbg
    k.stop_after = stop_after

    k.in_names = []

    def din(name, shape, dt=F32):
        need = {"xT": 3, "x_own": 6, "w_branch_a": 5, "w_branch_b": 5, "w_out": 6, "w_router": 6, "w_exp_gate": 8,
                "w_exp_up": 8, "w_exp_down": 8, "w_sh_gate": 7, "w_sh_up": 7, "w_sh_down": 9, "lnp": 6, "ropeK": 3}
        if stop_after < need.get(name, 0):
            shape = [1] * len(shape)
        k.in_names.append((name, tuple(shape)))
        return nc.dram_tensor(name, list(shape), dt, kind="ExternalInput").ap()

    k.xTh = din("xTh", [D, TH])
    k.xT = din("xT", [D, S])
    k.x_own = din("x_own", [TOK, D])
    k.w_in = din("w_in", [D, IN_COLS])
    k.w_a = din("w_branch_a", [NH * DH, D])
    k.w_b = din("w_branch_b", [NH * DH, D])
    k.w_out = din("w_out", [D, D])
    k.w_router = din("w_router", [D, E])
    k.w_eg = din("w_exp_gate", [E, D, FF])
    k.w_eu = din("w_exp_up", [E, D, FF])
    k.w_ed = din("w_exp_down", [E, FF, D])
    k.w_sg = din("w_sh_gate", [D, FF])
    k.w_su = din("w_sh_up", [D, FF])
    k.w_sd = din("w_sh_down", [FF, D])
    k.cpack = din("cpack", [128, CP_N])
    k.ppack = din("ppack", [128, PP_N])
    k.lnp = din("lnp", [128, 4, D])
    k.ropeK = din("ropeK", [S, 256])
    k.ropeQ = din("ropeQ", [TOK, 256])
    k.out = nc.dram_tensor("out", [TOK, D], F32, kind="ExternalOutput").ap()
    k.h1b = nc.dram_tensor("h1b_scr", [TOK + CAP, D], BF16).ap()
    k.acc = nc.dram_tensor("acc_scr", [TOK + CAP, D], F32).ap()
    k.dbgout = {}
    if dbg:
        for nm, shp, dt in (("d_oaT", [128, NH, TOK], BF16), ("d_obT", [128, NH, TOK], BF16),
                            ("d_mT", [128, 16, TOK], BF16), ("d_h1", [TOK, D], F32),
                            ("d_rt", [128, NT, 3, E], F32), ("d_idx", [128, E, 2], I32)):
            k.dbgout[nm] = nc.dram_tensor(nm, shp, dt, kind="ExternalOutput").ap()

    with ExitStack() as st:
        P = Prog(nc, st)
        k.P = P
        ARENA_BYTES = 207 * 1024
        raw = st.enter_context(nc.sbuf_tensor("arena", [128, ARENA_BYTES], U8))
        k.A = Arena(raw, ARENA_BYTES)
        k.pf = [st.enter_context(nc.psum_tensor(f"pf{i}", [128, 512], F32)) for i in range(6)]
        k.pb = [st.enter_context(nc.psum_tensor(f"pb{i}", [128, 1024], BF16)) for i in range(2)]
        stages = [stage0, stage1, stage2, stage3, stage4, stage5, stage6, stage7, stage7b, stage8]
        for i, fn in enumerate(stages):
            if i > stop_after:
                break
            fn(k)
        outs = [r for r in P.last_w if isinstance(r, tuple) and r[0] == "OUT"]
        P.op("sp", None, reads=outs)
        P.finalize()
        k.stats = P.stats
        k.peak = k.A.peak
    return nc, k


def cast_load_rows(k, dst3, src2, nk, ncols, res, k0=0):
    P = k.P
    srcv = src2.rearrange("(a p) c -> p a c", p=128)
    if ncols >= 1024:
        assert ncols % 1024 == 0
        for a in range(nk):
            for c0 in range(0, ncols, 1024):
                P.dma("pool", dst3[:, a, c0:c0 + 1024], srcv[:, k0 + a, c0:c0 + 1024], pwrites=[res])
    else:
        per = 1024 // ncols
        for a in range(0, nk, per):
            n = min(per, nk - a)
            P.dma("pool", dst3[:, a:a + n, :], srcv[:, k0 + a:k0 + a + n, :], pwrites=[res])


def evac(k, which, out, in_, reads, writes):
    P = k.P
    if which == "act":
        P.op("act", lambda e: e.copy(out=out, in_=in_), reads, writes)
    else:
        P.op("dve", lambda e: e.tensor_copy(out=out, in_=in_), reads, writes)


def stage0(k):
    P, A = k.P, k.A
    k.cp = A.alloc("cp", [128, CP_N], F32)
    k.pp = A.alloc("pp", [128, PP_N], F32)
    k.cst = A.alloc("cst", [128, 8], F32)
    k.ident_bf = A.alloc("ident_bf", [128, 128], BF16)
    k.ones_bf = A.alloc("ones_bf", [128, 128], BF16)
    k.u_bf = A.alloc("u_bf", [128, 128], BF16)
    P.dma("sp", k.cp[:, 0:640], k.cpack[:, 0:640], writes=["cp"])
    P.dma("sp", k.cp[:, 640:CP_N], k.cpack[:, 640:CP_N], writes=["cp2"])
    P.dma("sp", k.pp[:], k.ppack[:], writes=["pp"])
    P.op("dve", lambda e: e.memset(k.cst[:, 0:1], 0.0), writes=["cst"])
    P.op("dve", lambda e: e.memset(k.cst[:, 1:2], 1e-6), writes=["cst1"])
    P.op("dve", lambda e: e.memset(k.cst[:, 2:3], 1e-5), writes=["cst2"])
    P.op("dve", lambda e: e.memset(k.ones_bf[:], 1.0), writes=["ones_bf"])
    P.op("dve", lambda e: e.tensor_copy(out=k.ident_bf[:], in_=k.cp[:, CP_IDENT:CP_IDENT + 128]),
         reads=["cp"], writes=["ident_bf"])
    P.op("dve", lambda e: e.tensor_copy(out=k.u_bf[:], in_=k.cp[:, CP_U:CP_U + 128]), reads=["cp"],
         writes=["u_bf"])
    P.op("dve", lambda e: e.tensor_reduce(out=k.cst[:, 3:4], in_=k.pp[:, PP_GQ:PP_GQ + 128], axis=AX.X,
                                          op=ALU.max, apply_absolute_value=True), reads=["pp"], writes=["cst3"])
    P.op("dve", lambda e: e.tensor_reduce(out=k.cst[:, 4:5], in_=k.pp[:, PP_GK:PP_GK + 128], axis=AX.X,
                                          op=ALU.max, apply_absolute_value=True), reads=["pp"], writes=["cst4"])
    P.op("dve", lambda e: e.tensor_scalar(out=k.cst[:, 5:6], in0=k.cst[:, 3:4], scalar1=k.cst[:, 4:5],
                                          scalar2=-math.sqrt(DH), op0=ALU.mult, op1=ALU.mult),
         reads=["cst3", "cst4"], writes=["cst5"])


def norm_rope(k, src_ps, nh, gcol, ct, st_, dst, src_res, dst_res, tag):
    P = k.P
    junk, ssq, t0, t1, t2 = k.nr_junk, k.nr_ssq, k.nr_t0, k.nr_t1, k.nr_t2
    G = k.pp[:, gcol:gcol + 128]
    for h in range(nh):
        P.op("act", lambda e, h=h: e.activation(out=junk[:], in_=src_ps[:, h * 128:(h + 1) * 128], func=AF.Square,
                                                accum_out=ssq[:, h:h + 1]),
             reads=[src_res], writes=["nr_junk", ("nr_ssq", h)])
    P.op("act", lambda e: e.activation(out=ssq[:, 8:8 + nh], in_=ssq[:, 0:nh], func=AF.Sqrt, scale=1.0 / DH,
                                       bias=k.cst[:, 1:2]),
         reads=[("nr_ssq", h) for h in range(nh)] + ["cst1"], writes=["nr_std"])
    P.op("dve", lambda e: e.reciprocal(out=ssq[:, 16:16 + nh], in_=ssq[:, 8:8 + nh]), reads=["nr_std"],
         writes=["nr_rstd"])
    for h in range(nh):
        sl = slice(h * 128, (h + 1) * 128)
        P.op("dve", lambda e, h=h, sl=sl: e.scalar_tensor_tensor(out=t0[:], in0=src_ps[:, sl],
                                                                  scalar=ssq[:, 16 + h:17 + h], in1=G,
                                                                  op0=ALU.mult, op1=ALU.mult),
             reads=[src_res, "nr_rstd", "pp"], writes=["nr_t0"])
        P.op("dve", lambda e: e.tensor_tensor(out=t1[:], in0=t0[:], in1=ct, op=ALU.mult),
             reads=["nr_t0", tag], writes=["nr_t1"])
        t0v = t0.rearrange("p (a b j) -> p a b j", a=2, b=2)
        t2v = t2.rearrange("p (a b j) -> p a b j", a=2, b=2)
        sv = st_.rearrange("p (a b j) -> p a b j", a=2, b=2)
        P.op("dve", lambda e: e.tensor_tensor(out=t2v[:, :, 0, :], in0=t0v[:, :, 1, :], in1=sv[:, :, 0, :],
                                              op=ALU.mult), reads=["nr_t0", tag], writes=["nr_t2a"])
        P.op("dve", lambda e: e.tensor_tensor(out=t2v[:, :, 1, :], in0=t0v[:, :, 0, :], in1=sv[:, :, 1, :],
                                              op=ALU.mult), reads=["nr_t0", tag], writes=["nr_t2b"])
        P.op("dve", lambda e, sl=sl: e.tensor_tensor(out=dst[:, sl], in0=t1[:], in1=t2[:], op=ALU.add),
             reads=["nr_t1", "nr_t2a", "nr_t2b"], writes=[dst_res])


def stage1(k):
    P, A = k.P, k.A
    k.QbT = A.alloc("QbT", [128, NH, TOK], BF16)
    k.oaT = A.alloc("oaT", [128, NH, TOK], BF16)
    k.QaT = A.alloc("QaT", [128, NH, TOK], BF16)
    k.KaT = A.alloc("KaT", [128, NKV, TH], BF16)
    k.Va = A.alloc("Va", [128, TH // 128, NKV * DH], BF16)
    xTh = A.alloc("xTh", [128, 16, TH], BF16)
    wblk = [A.alloc(f"wblk{i}", [128, 16, 512], BF16) for i in range(2)]
    ropeq = A.alloc("ropeq", [128, NT, 256], F32)
    k.nr_junk = A.alloc("nr_junk", [128, 128], F32)
    k.nr_ssq = A.alloc("nr_ssq", [128, 24], F32)
    k.nr_t0 = A.alloc("nr_t0", [128, 128], F32)
    k.nr_t1 = A.alloc("nr_t1", [128, 128], F32)
    k.nr_t2 = A.alloc("nr_t2", [128, 128], F32)
    qn = [A.alloc(f"qn{i}", [128, 512], BF16) for i in range(2)]

    k.biasA = biasA = A.alloc("biasA", [128, NH, 384], F32)
    bmask = A.alloc("bmask", [128, 384], F32)
    idxA = k.cp[:, CP_IDXA:CP_IDXA + 384]
    maskA = k.cp[:, CP_MASKA:CP_MASKA + 384]
    for h in range(NH):
        P.op("dve", lambda e, h=h: e.tensor_copy(out=biasA[:, h, :], in_=maskA), reads=["cp2"],
             writes=[("biasA", h)])
    for b in range(32):
        P.op("dve", lambda e, b=b: e.tensor_scalar(out=bmask[:], in0=idxA, scalar1=float(b), scalar2=None,
                                                   op0=ALU.is_equal), reads=["cp", "cp2"], writes=["bmask"])
        for h in range(NH):
            P.op("dve", lambda e, b=b, h=h: e.scalar_tensor_tensor(
                out=biasA[:, h, :], in0=bmask[:], scalar=k.pp[:, PP_TAB + b * 8 + h:PP_TAB + b * 8 + h + 1],
                in1=biasA[:, h, :], op0=ALU.mult, op1=ALU.add), reads=["bmask", "pp", ("biasA", h)],
                writes=[("biasA", h)])
    P.dma("sp", ropeq[:], k.ropeQ.rearrange("(j p) c -> p j c", p=128), writes=["ropeq"])
    xv = k.xTh.rearrange("(a p) c -> p a c", p=128)
    for a in range(16):
        for hf in range(2):
            P.dma("pool", xTh[:, a, hf * 640:(hf + 1) * 640], xv[:, a, hf * 640:(hf + 1) * 640],
                  pwrites=[("xTh", a)])
    xres = [("xTh", a) for a in range(16)]
    blocks = [("qa", C_QA), ("qa", C_QA + 512), ("kv", C_KA), ("qb", C_QB), ("qb", C_QB + 512)]

    def load_blk(bi):
        kind, c0 = blocks[bi]
        cast_load_rows(k, wblk[bi % 2], k.w_in[:, c0:c0 + 512], 16, 512, ("wblk", bi % 2))

    load_blk(0)
    pfi = [0]
    evi = [0]

    def nextpf():
        pfi[0] = (pfi[0] + 1) % 4
        return pfi[0]

    def nextev():
        evi[0] ^= 1
        return "act" if evi[0] else "dve"

    for bi, (kind, c0) in enumerate(blocks):
        if bi + 1 < len(blocks):
            load_blk(bi + 1)
        w = wblk[bi % 2]
        wr = ("wblk", bi % 2)
        if kind == "qa":
            hb = (c0 - C_QA) // 128
            for hh in range(4):
                for tg in range(2):
                    b = nextpf()
                    ps = k.pf[b]
                    for a in range(16):
                        P.op("pe", lambda e, a=a, hh=hh, tg=tg, ps=ps, w=w: e.matmul(
                            ps[:, :], w[:, a, hh * 128:(hh + 1) * 128],
                            xTh[:, a, HALO + tg * 512:HALO + (tg + 1) * 512], start=(a == 0), stop=(a == 15)),
                            reads=[wr, ("xTh", a)], writes=[("pf", b)])
                    evac(k, nextev(), k.QaT[:, hb + hh, tg * 512:(tg + 1) * 512], ps[:, :], [("pf", b)],
                         [("QaT", hb + hh)])
        elif kind == "kv":
            for kvh in range(NKV):
                for g0, g1 in ((0, 512), (512, 1024), (1024, TH)):
                    b = nextpf()
                    ps = k.pf[b]
                    for a in range(16):
                        P.op("pe", lambda e, a=a, kvh=kvh, g0=g0, g1=g1, ps=ps, w=w: e.matmul(
                            ps[:, 0:g1 - g0], w[:, a, kvh * 128:(kvh + 1) * 128], xTh[:, a, g0:g1],
                            start=(a == 0), stop=(a == 15)), reads=[wr, ("xTh", a)], writes=[("pf", b)])
                    evac(k, nextev(), k.KaT[:, kvh, g0:g1], ps[:, 0:g1 - g0], [("pf", b)], ["KaT"])
            for t in range(TH // 128):
                b = nextpf()
                ps = k.pf[b]
                for a in range(16):
                    P.op("pe", lambda e, a=a, t=t, ps=ps, w=w: e.matmul(
                        ps[:, 0:256], xTh[:, a, t * 128:(t + 1) * 128], w[:, a, 256:512],
                        start=(a == 0), stop=(a == 15)), reads=[wr, ("xTh", a)], writes=[("pf", b)])
                evac(k, nextev(), k.Va[:, t, :], ps[:, 0:256], [("pf", b)], ["Va"])
        else:
            hb = (c0 - C_QB) // 128
            for i in range(NT):
                b = nextpf()
                ps = k.pf[b]
                for a in range(16):
                    P.op("pe", lambda e, a=a, i=i, ps=ps, w=w: e.matmul(
                        ps[:, :], xTh[:, a, HALO + i * 128:HALO + (i + 1) * 128], w[:, a, :],
                        start=(a == 0), stop=(a == 15)), reads=[wr, ("xTh", a)], writes=[("pf", b)])
                q = qn[i % 2]
                qres = ("qn", i % 2)
                norm_rope(k, ps, 4, PP_GQ, ropeq[:, i, 0:128], ropeq[:, i, 128:256], q, ("pf", b), qres, "ropeq")
                pb = k.pb[i % 2]
                for hh in range(4):
                    P.op("pe", lambda e, hh=hh, pb=pb, q=q: e.transpose(pb[:, hh * 128:(hh + 1) * 128],
                                                                        q[:, hh * 128:(hh + 1) * 128],
                                                                        k.ident_bf[:]),
                         reads=[qres, "ident_bf"], writes=[("pb", i % 2)])
                evac(k, "act", k.QbT[:, hb:hb + 4, i * 128:(i + 1) * 128],
                     pb[:, 0:512].rearrange("p (h t) -> p h t", h=4), [("pb", i % 2)], ["QbT"])
    P.barrier()
    A.release("xTh", "wblk0", "wblk1", "ropeq", "qn0", "qn1")


def stage2(k):
    P, A = k.P, k.A
    biasA = k.biasA
    ssb = [A.alloc(f"ssb{i}", [128, 384], F32) for i in range(2)]
    pfp = [A.alloc(f"pfp{i}", [128, 384], F32) for i in range(2)]
    pnb = [A.alloc(f"pnb{i}", [128, 384], BF16) for i in range(2)]
    pTs = [A.alloc(f"pTs{i}", [128, 384], BF16) for i in range(2)]
    stt = A.alloc("stt", [128, 2, 8], F32)
    units = [(n, kvh, hh) for n in range(NT) for kvh in range(NKV) for hh in range(4)]

    def part_a(u):
        n, kvh, hh = units[u]
        h = kvh * 4 + hh
        sb_i = u % 2
        sps = k.pf[sb_i]
        s_sb, p_f = ssb[sb_i], pfp[sb_i]
        R = lambda nm: (nm, sb_i)
        sv = stt[:, sb_i, :]
        P.op("pe", lambda e: e.matmul(sps[:, 0:384], k.QaT[:, h, n * 128:(n + 1) * 128],
                                      k.KaT[:, kvh, n * 128:n * 128 + 384], start=True, stop=True),
             reads=[("QaT", h), "KaT"], writes=[("pf", sb_i)])
        P.op("dve", lambda e: e.scalar_tensor_tensor(out=s_sb[:], in0=sps[:, 0:384], scalar=SCALE, in1=biasA[:, h, :],
                                                     op0=ALU.mult, op1=ALU.add),
             reads=[("pf", sb_i), ("biasA", h)], writes=[R("ssb")])
        if n == 0:
            P.op("dve", lambda e: e.tensor_scalar(out=s_sb[:, 0:128], in0=s_sb[:, 0:128],
                                                  scalar1=k.pp[:, PP_EDGE:PP_EDGE + 1], scalar2=None, op0=ALU.add),
                 reads=[R("ssb"), "pp"], writes=[R("ssb")])
        if n == NT - 1:
            P.op("dve", lambda e: e.tensor_scalar(out=s_sb[:, 256:384], in0=s_sb[:, 256:384],
                                                  scalar1=k.pp[:, PP_EDGE + 1:PP_EDGE + 2], scalar2=None, op0=ALU.add),
                 reads=[R("ssb"), "pp"], writes=[R("ssb")])
        P.op("dve", lambda e: e.reduce_max(out=sv[:, 0:1], in_=s_sb[:], axis=AX.X), reads=[R("ssb")], writes=[R("st0")])
        P.op("dve", lambda e: e.tensor_scalar(out=sv[:, 1:2], in0=sv[:, 0:1],
                                              scalar1=k.pp[:, PP_SINK + h:PP_SINK + h + 1], scalar2=-1.0,
                                              op0=ALU.max, op1=ALU.mult), reads=[R("st0"), "pp"], writes=[R("st1")])
        P.op("act", lambda e: e.activation(out=p_f[:], in_=s_sb[:], func=AF.Exp, bias=sv[:, 1:2], scale=1.0,
                                           accum_out=sv[:, 2:3]),
             reads=[R("ssb"), R("st1")], writes=[R("pfp"), R("st2")])
        P.op("act", lambda e: e.activation(out=sv[:, 3:4], in_=k.pp[:, PP_SINK + h:PP_SINK + h + 1], func=AF.Exp,
                                           bias=sv[:, 1:2], scale=1.0), reads=[R("st1"), "pp"], writes=[R("st3")])

    def part_b(u):
        n, kvh, hh = units[u]
        ob = 4 + kvh
        sb_i = u % 2
        p_f, p_n, pT = pfp[sb_i], pnb[sb_i], pTs[sb_i]
        R = lambda nm: (nm, sb_i)
        sv = stt[:, sb_i, :]
        P.op("dve", lambda e: e.tensor_tensor(out=sv[:, 4:5], in0=sv[:, 2:3], in1=sv[:, 3:4], op=ALU.add),
             reads=[R("st2"), R("st3")], writes=[R("st4")])
        P.op("dve", lambda e: e.reciprocal(out=sv[:, 5:6], in_=sv[:, 4:5]), reads=[R("st4")], writes=[R("st5")])
        P.op("dve", lambda e: e.tensor_scalar(out=p_n[:], in0=p_f[:], scalar1=sv[:, 5:6], scalar2=None, op0=ALU.mult),
             reads=[R("pfp"), R("st5")], writes=[R("pnb")])
        pb = k.pb[sb_i]
        for j in range(3):
            P.op("pe", lambda e, j=j: e.transpose(pb[:, j * 128:(j + 1) * 128], p_n[:, j * 128:(j + 1) * 128],
                                                  k.ident_bf[:]),
                 reads=[R("pnb"), "ident_bf"], writes=[("pb", sb_i)])
        evac(k, "act", pT[:], pb[:, 0:384], [("pb", sb_i)], [R("pTs")])
        ops_ = k.pf[ob]
        for j in range(3):
            P.op("pe", lambda e, j=j: e.matmul(ops_[:, hh * 128:(hh + 1) * 128],
                                               k.Va[:, n + j, kvh * 128:(kvh + 1) * 128],
                                               pT[:, j * 128:(j + 1) * 128], start=(j == 0), stop=(j == 2)),
                 reads=["Va", R("pTs")], writes=[("pf", ob)])
        if hh == 3:
            evac(k, "dve", k.oaT[:, kvh * 4:(kvh + 1) * 4, n * 128:(n + 1) * 128],
                 k.pf[ob][:, :].rearrange("p (h t) -> p h t", h=4), [("pf", ob)], ["oaT"])

    part_a(0)
    for u in range(1, len(units)):
        part_a(u)
        part_b(u - 1)
    part_b(len(units) - 1)
    if k.dbg:
        P.dma("sp", k.dbgout["d_oaT"][:], k.oaT[:], reads=["oaT"], writes=[("OUT", "d_oaT")])
    P.barrier()
    A.release("biasA", "bmask", "ssb0", "ssb1", "pfp0", "pfp1", "pnb0", "pnb1", "pTs0", "pTs1", "stt",
              "QaT", "KaT", "Va")


def stage3(k):
    P, A = k.P, k.A
    k.obT = A.alloc("obT", [128, NH, TOK], BF16)
    k.KbT = A.alloc("KbT", [128, NKV, S], BF16)
    k.Vb = A.alloc("Vb", [128, S // 128, NKV * DH], BF16)
    wkv = A.alloc("wkv", [128, 16, 512], BF16)
    xc = [A.alloc(f"xc{i}", [128, 16, 512], BF16) for i in range(2)]
    rk = [A.alloc(f"rk{i}", [128, 4, 256], F32) for i in range(2)]
    kn = [A.alloc(f"kn{i}", [128, 256], BF16) for i in range(2)]
    cast_load_rows(k, wkv, k.w_in[:, C_KB:C_KB + 512], 16, 512, "wkv")
    xTv = k.xT.rearrange("(a p) c -> p a c", p=128)
    NG = S // 512

    def load(g):
        sl = g % 2
        for a in range(0, 16, 2):
            P.dma("pool", xc[sl][:, a:a + 2, :], xTv[:, a:a + 2, g * 512:(g + 1) * 512], writes=[("xc", sl, a)])
        P.dma("sp", rk[sl][:], k.ropeK[g * 512:(g + 1) * 512, :].rearrange("(j p) c -> p j c", p=128),
              writes=[("rk", sl)])

    def finish(tt):
        q = kn[tt % 2]
        qres = ("kn", tt % 2)
        pb = k.pb[tt % 2]
        for h in range(2):
            P.op("pe", lambda e, h=h: e.transpose(pb[:, h * 128:(h + 1) * 128], q[:, h * 128:(h + 1) * 128],
                                                  k.ident_bf[:]),
                 reads=[qres, "ident_bf"], writes=[("pb", tt % 2)])
        evac(k, "act", k.KbT[:, 0:2, tt * 128:(tt + 1) * 128],
             pb[:, 0:256].rearrange("p (h t) -> p h t", h=2), [("pb", tt % 2)], [("KbT", tt)])

    load(0)
    for g in range(NG):
        if g + 1 < NG:
            load(g + 1)
        sl = g % 2
        for j in range(4):
            tt = g * 4 + j
            b = tt % 4
            ps = k.pf[b]
            for a in range(16):
                P.op("pe", lambda e, a=a, j=j, ps=ps, sl=sl: e.matmul(
                    ps[:, :], xc[sl][:, a, j * 128:(j + 1) * 128], wkv[:, a, :], start=(a == 0), stop=(a == 15)),
                    reads=["wkv", ("xc", sl, a - a % 2)], writes=[("pf", b)])
            if tt >= 1:
                finish(tt - 1)
            evac(k, "act", k.Vb[:, tt, :], ps[:, 256:512], [("pf", b)], [("Vb", tt)])
            norm_rope(k, ps, 2, PP_GK, rk[sl][:, j, 0:128], rk[sl][:, j, 128:256], kn[tt % 2], ("pf", b),
                      ("kn", tt % 2), ("rk", sl))
    finish(S // 128 - 1)
    P.barrier()
    A.release("wkv", "xc0", "xc1", "rk0", "rk1", "kn0", "kn1")


def stage4(k):
    P, A = k.P, k.A
    pT = [A.alloc(f"pT{i}", [128, 512], BF16) for i in range(3)]
    rden = [A.alloc(f"rden{i}", [128, 512], F32) for i in range(2)]
    NKT = S // 128
    kres = [("KbT", t) for t in range(NKT)]
    vres = [("Vb", t) for t in range(NKT)]
    it = 0
    for qg in range(2):
        for h in range(NH):
            kvh = h // 4
            bo, bd = (2, 3) if it % 2 == 0 else (4, 5)
            accO, accD = k.pf[bo], k.pf[bd]
            qsl = slice(qg * 512, (qg + 1) * 512)

            def pv(kt, accO=accO, accD=accD, kvh=kvh, bo=bo, bd=bd):
                pt = pT[kt % 3]
                P.op("pe", lambda e, kt=kt, pt=pt, accO=accO, kvh=kvh: e.matmul(
                    accO[:, :], k.Vb[:, kt, kvh * 128:(kvh + 1) * 128], pt[:], start=(kt == 0), stop=(kt == NKT - 1)),
                    reads=[("pT", kt % 3), vres[kt]], writes=[("pf", bo)])
                P.op("pe", lambda e, kt=kt, pt=pt, accD=accD: e.matmul(accD[:, :], k.ones_bf[:], pt[:],
                                                                       start=(kt == 0), stop=(kt == NKT - 1)),
                     reads=[("pT", kt % 3), "ones_bf"], writes=[("pf", bd)])

            for kt in range(NKT):
                sb = kt % 2
                ST = k.pf[sb]
                P.op("pe", lambda e, kt=kt, ST=ST, kvh=kvh, h=h, qsl=qsl: e.matmul(
                    ST[:, :], k.KbT[:, kvh, kt * 128:(kt + 1) * 128], k.QbT[:, h, qsl], start=True, stop=True),
                     reads=[kres[kt], "QbT"], writes=[("pf", sb)])
                P.op("act", lambda e, kt=kt, ST=ST: e.activation(out=pT[kt % 3][:], in_=ST[:, :], func=AF.Exp,
                                                                 bias=k.cst[:, 5:6], scale=SCALE),
                     reads=[("pf", sb), "cst5"], writes=[("pT", kt % 3)])
                if kt >= 1:
                    pv(kt - 1)
            pv(NKT - 1)
            rd = rden[it % 2]
            P.op("dve", lambda e, rd=rd, accD=accD: e.reciprocal(out=rd[:], in_=accD[:, :]), reads=[("pf", bd)],
                 writes=[("rden", it % 2)])
            P.op("dve", lambda e, rd=rd, accO=accO, h=h, qsl=qsl: e.tensor_tensor(
                out=k.obT[:, h, qsl], in0=accO[:, :], in1=rd[:], op=ALU.mult),
                reads=[("pf", bo), ("rden", it % 2)], writes=["obT"])
            it += 1
    if k.dbg:
        P.dma("sp", k.dbgout["d_obT"][:], k.obT[:], reads=["obT"], writes=[("OUT", "d_obT")])
    P.barrier()
    A.release("pT0", "pT1", "pT2", "rden0", "rden1", "KbT", "Vb", "QbT")


def stage5(k):
    P, A = k.P, k.A
    k.mT = A.alloc("mT", [128, 16, TOK], BF16)
    xo = A.alloc("xo", [128, 16, TOK], BF16)
    wga = [A.alloc(f"wga{i}", [128, 16, 256], BF16) for i in range(2)]
    wgb = [A.alloc(f"wgb{i}", [128, 16, 256], BF16) for i in range(2)]
    wa = [A.alloc(f"wa{i}", [128, 8, 256], BF16) for i in range(2)]
    wb = [A.alloc(f"wb{i}", [128, 8, 256], BF16) for i in range(2)]
    sga = [A.alloc(f"sga{i}", [128, 512], F32) for i in range(2)]
    sgb = [A.alloc(f"sgb{i}", [128, 512], F32) for i in range(2)]
    tma = [A.alloc(f"tma{i}", [128, 512], F32) for i in range(2)]
    tmb = [A.alloc(f"tmb{i}", [128, 512], F32) for i in range(2)]
    xv = k.xTh.rearrange("(a p) c -> p a c", p=128)
    for a in range(16):
        P.dma("pool", xo[:, a, :], xv[:, a, HALO:HALO + TOK], writes=[("xo", a)])

    def load(cb):
        sl = cb % 2
        cast_load_rows(k, wga[sl], k.w_in[:, C_GA + cb * 256:C_GA + (cb + 1) * 256], 16, 256, ("wga", sl))
        cast_load_rows(k, wgb[sl], k.w_in[:, C_GB + cb * 256:C_GB + (cb + 1) * 256], 16, 256, ("wgb", sl))
        cast_load_rows(k, wa[sl], k.w_a[:, cb * 256:(cb + 1) * 256], 8, 256, ("wa", sl))
        cast_load_rows(k, wb[sl], k.w_b[:, cb * 256:(cb + 1) * 256], 8, 256, ("wb", sl))

    load(0)
    it = 0
    for cb in range(8):
        if cb + 1 < 8:
            load(cb + 1)
        sl = cb % 2
        for jj in range(2):
            j = cb * 2 + jj
            cs = slice(jj * 128, (jj + 1) * 128)
            for tg in range(2):
                ts_ = slice(tg * 512, (tg + 1) * 512)
                banks = [(4 * it + r) % 6 for r in range(4)]
                pa, pbr, pga, pgb = (k.pf[b] for b in banks)
                s2 = it % 2
                for h in range(NH):
                    P.op("pe", lambda e, h=h, pa=pa, sl=sl, cs=cs, ts_=ts_: e.matmul(
                        pa[:, :], wa[sl][:, h, cs], k.oaT[:, h, ts_], start=(h == 0), stop=(h == NH - 1)),
                        reads=[("wa", sl), "oaT"], writes=[("pf", banks[0])])
                for h in range(NH):
                    P.op("pe", lambda e, h=h, pbr=pbr, sl=sl, cs=cs, ts_=ts_: e.matmul(
                        pbr[:, :], wb[sl][:, h, cs], k.obT[:, h, ts_], start=(h == 0), stop=(h == NH - 1)),
                        reads=[("wb", sl), "obT"], writes=[("pf", banks[1])])
                for a in range(16):
                    P.op("pe", lambda e, a=a, pga=pga, sl=sl, cs=cs, ts_=ts_: e.matmul(
                        pga[:, :], wga[sl][:, a, cs], xo[:, a, ts_], start=(a == 0), stop=(a == 15)),
                        reads=[("wga", sl), ("xo", a)], writes=[("pf", banks[2])])
                for a in range(16):
                    P.op("pe", lambda e, a=a, pgb=pgb, sl=sl, cs=cs, ts_=ts_: e.matmul(
                        pgb[:, :], wgb[sl][:, a, cs], xo[:, a, ts_], start=(a == 0), stop=(a == 15)),
                        reads=[("wgb", sl), ("xo", a)], writes=[("pf", banks[3])])
                P.op("act", lambda e, pga=pga, s2=s2, j=j: e.activation(
                    out=sga[s2][:], in_=pga[:, :], func=AF.Sigmoid, bias=k.pp[:, PP_BG + j:PP_BG + j + 1], scale=1.0),
                    reads=[("pf", banks[2]), "pp"], writes=[("sga", s2)])
                P.op("act", lambda e, pgb=pgb, s2=s2, j=j: e.activation(
                    out=sgb[s2][:], in_=pgb[:, :], func=AF.Sigmoid, bias=k.pp[:, PP_BG + 16 + j:PP_BG + 17 + j],
                    scale=1.0), reads=[("pf", banks[3]), "pp"], writes=[("sgb", s2)])
                P.op("dve", lambda e, pa=pa, s2=s2: e.tensor_tensor(out=tma[s2][:], in0=pa[:, :], in1=sga[s2][:],
                                                                   op=ALU.mult),
                     reads=[("pf", banks[0]), ("sga", s2)], writes=[("tma", s2)])
                P.op("dve", lambda e, pbr=pbr, s2=s2: e.tensor_tensor(out=tmb[s2][:], in0=pbr[:, :], in1=sgb[s2][:],
                                                                     op=ALU.mult),
                     reads=[("pf", banks[1]), ("sgb", s2)], writes=[("tmb", s2)])
                P.op("dve", lambda e, s2=s2, j=j, ts_=ts_: e.tensor_tensor(out=k.mT[:, j, ts_], in0=tma[s2][:],
                                                                           in1=tmb[s2][:], op=ALU.add),
                     reads=[("tma", s2), ("tmb", s2)], writes=[("mT", j)])
                it += 1
    if k.dbg:
        P.dma("sp", k.dbgout["d_mT"][:], k.mT[:], reads=[("mT", j) for j in range(16)], writes=[("OUT", "d_mT")])
    P.barrier()
    A.release("xo", "wga0", "wga1", "wgb0", "wgb1", "wa0", "wa1", "wb0", "wb1", "sga0", "sga1", "sgb0", "sgb1",
              "tma0", "tma1", "tmb0", "tmb1", "oaT", "obT")
    A.release("nr_junk", "nr_ssq", "nr_t0", "nr_t1", "nr_t2")


def layer_norm_tile(k, xt, xres, lng, lnres, lnst, mv, tag):
    P = k.P
    P.op("dve", lambda e: e.bn_aggr(out=mv[:, 0:2], in_=lnst.rearrange("p a b -> p (a b)")),
         reads=[(tag, "st", d) for d in range(4)], writes=[(tag, "mv")])
    P.op("act", lambda e: e.activation(out=mv[:, 2:3], in_=mv[:, 1:2], func=AF.Sqrt, bias=k.cst[:, 2:3], scale=1.0),
         reads=[(tag, "mv"), "cst2"], writes=[(tag, "sd")])
    P.op("dve", lambda e: e.reciprocal(out=mv[:, 3:4], in_=mv[:, 2:3]), reads=[(tag, "sd")], writes=[(tag, "rs")])
    P.op("dve", lambda e: e.tensor_scalar(out=xt[:], in0=xt[:], scalar1=mv[:, 0:1], scalar2=mv[:, 3:4],
                                          op0=ALU.subtract, op1=ALU.mult), reads=[xres, (tag, "mv"), (tag, "rs")],
         writes=[xres])
    P.op("dve", lambda e: e.tensor_tensor(out=xt[:], in0=xt[:], in1=lng[:, 0, :], op=ALU.mult), reads=[xres, lnres],
         writes=[xres])
    P.op("dve", lambda e: e.tensor_tensor(out=xt[:], in0=xt[:], in1=lng[:, 1, :], op=ALU.add), reads=[xres, lnres],
         writes=[xres])


def stage6(k):
    P, A = k.P, k.A
    k.h1T = A.alloc("h1T", [128, 16, TOK], BF16)
    k.rtM = A.alloc("rtM", [128, NT, E], F32)
    k.rtW = A.alloc("rtW", [128, NT, E], F32)
    wo = A.alloc("wo", [128, 16, D], BF16)
    lng = A.alloc("lng", [128, 2, D], F32)
    xts = [A.alloc(f"xt{i}", [128, D], F32) for i in range(2)]
    racc = A.alloc("racc", [128, D], F32)
    h1bf = A.alloc("h1bf", [128, D], BF16)
    h1Tf = A.alloc("h1Tf", [128, 16, 128], F32)
    wr = A.alloc("wr", [128, 16, E], F32)
    lnst = A.alloc("lnst", [128, 4, 6], F32)
    mv = A.alloc("mv", [128, 8], F32)
    rs = A.alloc("rs", [128, 12, E], F32)
    r8 = A.alloc("r8", [128, 12, 8], F32)
    cmp3 = A.alloc("cmp3", [128, 8, 8], F32)
    ident_f = k.cp[:, CP_IDENT:CP_IDENT + 128]

    cast_load_rows(k, wo, k.w_out, 16, D, "wo")
    P.dma("sp", lng[:], k.lnp[:, 0:2, :], writes=["lng"])
    P.dma("sp", wr[:], k.w_router.rearrange("(a p) c -> p a c", p=128), writes=["wr"])
    P.op("dve", lambda e: e.memset(h1bf[:], 0.0), writes=["h1bf"])
    for z in range(CAP // 128):
        P.dma("sp", k.h1b[TOK + z * 128:TOK + (z + 1) * 128, :], h1bf[:], reads=["h1bf"], writes=[("H1BZ", z)])
    for i in range(NT):
        xt = xts[i % 2]
        xres = ("xt", i % 2)
        tsl = slice(i * 128, (i + 1) * 128)
        P.dma("sp", xt[:], k.x_own[tsl, :], writes=[xres])
        for dg in range(4):
            b = dg % 2
            ps = k.pf[b]
            dsl = slice(dg * 512, (dg + 1) * 512)
            for j in range(16):
                P.op("pe", lambda e, j=j, ps=ps, tsl=tsl, dsl=dsl: e.matmul(
                    ps[:, :], k.mT[:, j, tsl], wo[:, j, dsl], start=(j == 0), stop=(j == 15)),
                    reads=[("mT", j), "wo"], writes=[("pf", b)])
            P.op("dve", lambda e, ps=ps, xt=xt, dsl=dsl: e.scalar_tensor_tensor(
                out=xt[:, dsl], in0=xt[:, dsl], scalar=ALPHA, in1=ps[:, :], op0=ALU.mult, op1=ALU.add),
                reads=[xres, ("pf", b)], writes=[xres])
            P.op("dve", lambda e, xt=xt, dsl=dsl, dg=dg: e.bn_stats(out=lnst[:, dg, :], in_=xt[:, dsl]),
                 reads=[xres], writes=[("ln1", "st", dg)])
        layer_norm_tile(k, xt, xres, lng, "lng", lnst, mv, "ln1")
        P.op("act", lambda e, xt=xt: e.copy(out=h1bf[:], in_=xt[:]), reads=[xres], writes=["h1bf"])
        P.dma("sp", k.h1b[tsl, :], h1bf[:], reads=["h1bf"], writes=[("H1B", i)])
        P.op("act", lambda e, xt=xt: e.mul(out=racc[:], in_=xt[:], mul=ALPHA), reads=[xres], writes=["racc"])
        P.dma("sp", k.acc[tsl, :], racc[:], reads=["racc"], writes=[("ACCI", i)])
        if k.dbg:
            P.dma("sp", k.dbgout["d_h1"][tsl, :], xt[:], reads=[xres], writes=[("OUT", "d_h1", i)])
        for q4 in range(4):
            b = 2 + q4
            ps = k.pf[b]
            for r in range(4):
                a = q4 * 4 + r
                P.op("pe", lambda e, a=a, r=r, ps=ps, xt=xt: e.transpose(
                    ps[:, r * 128:(r + 1) * 128], xt[:, a * 128:(a + 1) * 128], ident_f),
                    reads=[xres, "cp"], writes=[("pf", b)])
            P.op("act", lambda e, q4=q4, ps=ps: e.copy(out=h1Tf[:, q4 * 4:(q4 + 1) * 4, :],
                                                       in_=ps[:, :].rearrange("p (r t) -> p r t", r=4)),
                 reads=[("pf", b)], writes=[("h1Tf", q4), ("pf", b)])
            P.op("dve", lambda e, q4=q4, ps=ps, tsl=tsl: e.tensor_copy(
                out=k.h1T[:, q4 * 4:(q4 + 1) * 4, tsl], in_=ps[:, :].rearrange("p (r t) -> p r t", r=4)),
                reads=[("pf", b)], writes=[("h1T", i)])
        lg = k.pf[0]
        for a in range(16):
            P.op("pe", lambda e, a=a: e.matmul(lg[:, 0:E], h1Tf[:, a, :], wr[:, a, :], start=(a == 0), stop=(a == 15)),
                 reads=[("h1Tf", a // 4), "wr"], writes=[("pf", 0)])
        sc, bz, eq, mk2, mvv, ws = (rs[:, r, :] for r in range(6))
        m1, m2, gs, cnt, gsel, pen, top8, wsum, rw = (r8[:, r, :] for r in range(9))
        v3 = lambda ap: ap.rearrange("p (g j) -> p g j", g=8)
        P.op("act", lambda e: e.activation(out=sc, in_=lg[:, 0:E], func=AF.Sigmoid), reads=[("pf", 0)], writes=["r_sc"])
        P.op("dve", lambda e: e.tensor_tensor(out=bz, in0=sc, in1=k.pp[:, PP_RB:PP_RB + E], op=ALU.add),
             reads=["r_sc", "pp"], writes=["r_bz"])
        P.op("dve", lambda e: e.reduce_max(out=m1, in_=v3(bz), axis=AX.X), reads=["r_bz"], writes=["r_m1"])
        P.op("dve", lambda e: e.tensor_tensor(out=v3(eq), in0=v3(bz), in1=m1.unsqueeze(2).to_broadcast([128, 8, 8]),
                                              op=ALU.is_equal), reads=["r_bz", "r_m1"], writes=["r_eq"])
        P.op("dve", lambda e: e.scalar_tensor_tensor(out=mk2, in0=eq, scalar=-BIG, in1=bz, op0=ALU.mult, op1=ALU.add),
             reads=["r_eq", "r_bz"], writes=["r_mk2"])
        P.op("dve", lambda e: e.reduce_max(out=m2, in_=v3(mk2), axis=AX.X), reads=["r_mk2"], writes=["r_m2"])
        P.op("dve", lambda e: e.tensor_tensor(out=gs, in0=m1, in1=m2, op=ALU.add), reads=["r_m1", "r_m2"],
             writes=["r_gs"])
        P.op("dve", lambda e: e.tensor_tensor(out=cmp3[:], in0=gs.unsqueeze(1).to_broadcast([128, 8, 8]),
                                              in1=gs.unsqueeze(2).to_broadcast([128, 8, 8]), op=ALU.is_gt),
             reads=["r_gs"], writes=["r_cmp"])
        P.op("dve", lambda e: e.reduce_sum(out=cnt, in_=cmp3[:], axis=AX.X), reads=["r_cmp"], writes=["r_cnt"])
        P.op("dve", lambda e: e.tensor_scalar(out=gsel, in0=cnt, scalar1=3.5, scalar2=None, op0=ALU.is_lt),
             reads=["r_cnt"], writes=["r_gsel"])
        P.op("dve", lambda e: e.tensor_scalar(out=pen, in0=gsel, scalar1=BIG, scalar2=-BIG, op0=ALU.mult, op1=ALU.add),
             reads=["r_gsel"], writes=["r_pen"])
        P.op("dve", lambda e: e.tensor_tensor(out=v3(mvv), in0=v3(bz), in1=gsel.unsqueeze(2).to_broadcast([128, 8, 8]),
                                              op=ALU.mult), reads=["r_bz", "r_gsel"], writes=["r_mv"])
        P.op("dve", lambda e: e.tensor_tensor(out=v3(mvv), in0=v3(mvv), in1=pen.unsqueeze(2).to_broadcast([128, 8, 8]),
                                              op=ALU.add), reads=["r_mv", "r_pen"], writes=["r_mv"])
        P.op("dve", lambda e: e.max(out=top8, in_=mvv), reads=["r_mv"], writes=["r_top8"])
        P.op("dve", lambda e, i=i: e.tensor_scalar(out=k.rtM[:, i, :], in0=mvv, scalar1=top8[:, 7:8], scalar2=None,
                                                   op0=ALU.is_ge), reads=["r_mv", "r_top8"], writes=[("rtM", i)])
        P.op("dve", lambda e, i=i: e.tensor_tensor(out=ws, in0=sc, in1=k.rtM[:, i, :], op=ALU.mult),
             reads=["r_sc", ("rtM", i)], writes=["r_ws"])
        P.op("dve", lambda e: e.reduce_sum(out=wsum[:, 0:1], in_=ws, axis=AX.X), reads=["r_ws"], writes=["r_wsum"])
        P.op("dve", lambda e: e.reciprocal(out=rw[:, 0:1], in_=wsum[:, 0:1]), reads=["r_wsum"], writes=["r_rw"])
        P.op("dve", lambda e, i=i: e.tensor_scalar(out=k.rtW[:, i, :], in0=ws, scalar1=rw[:, 0:1], scalar2=2.5,
                                                   op0=ALU.mult, op1=ALU.mult), reads=["r_ws", "r_rw"],
             writes=[("rtW", i)])
    P.barrier()
    A.release("wo", "lng", "xt0", "xt1", "racc", "h1bf", "h1Tf", "wr", "lnst", "mv", "rs", "r8", "cmp3", "mT")


def stage7(k):
    P, A = k.P, k.A
    Mbf = A.alloc("Mbf", [128, NT, E], BF16)
    rankp = A.alloc("rankp", [128, NT, E], F32)
    VW = A.alloc("VW", [128, NT, E, 3], F32)
    k.idx = A.alloc("idx", [128, E, 2], I32)
    k.wsl = A.alloc("wsl", [128, E, 2], F32)
    Sb = [A.alloc(f"Sb{i}", [128, CAP], F32) for i in range(2 * NT)]
    padv = A.alloc("padv", [128, 2], F32)
    k.hsT = A.alloc("hsT", [128, 4, TOK], BF16)
    wsg = A.alloc("wsg", [128, 16, FF], BF16)
    wsu = A.alloc("wsu", [128, 16, FF], BF16)
    sgs = [A.alloc(f"sgs{i}", [128, 512], F32) for i in range(2)]
    cast_load_rows(k, wsg, k.w_sg, 16, FF, "wsg")
    cast_load_rows(k, wsu, k.w_su, 16, FF, "wsu")
    iota = k.cp[:, CP_IOTA:CP_IOTA + CAP]
    for i in range(NT):
        P.op("dve", lambda e, i=i: e.tensor_copy(out=Mbf[:, i, :], in_=k.rtM[:, i, :]), reads=[("rtM", i)],
             writes=[("Mbf", i)])
        P.op("dve", lambda e, i=i: e.tensor_copy(out=VW[:, i, :, 0],
                                                 in_=k.cp[:, CP_VCOL + i:CP_VCOL + i + 1].to_broadcast([128, E])),
             reads=["cp2"], writes=[("VW0", i)])
        P.op("dve", lambda e, i=i: e.tensor_copy(out=VW[:, i, :, 1], in_=k.rtW[:, i, :]), reads=[("rtW", i)],
             writes=[("VW1", i)])
        P.op("dve", lambda e, i=i: e.memset(VW[:, i, :, 2], 1.0), writes=[("VW2", i)])
    for j in range(NT):
        ps = k.pf[4]
        for i in range(j):
            P.op("pe", lambda e, i=i, j=j: e.matmul(ps[:, 0:E], k.ones_bf[:], Mbf[:, i, :], start=(i == 0), stop=False),
                 reads=["ones_bf", ("Mbf", i)], writes=[("pf", 4)])
        P.op("pe", lambda e, j=j: e.matmul(ps[:, 0:E], k.u_bf[:], Mbf[:, j, :], start=(j == 0), stop=True),
             reads=["u_bf", ("Mbf", j)], writes=[("pf", 4)])
        P.op("dve", lambda e, j=j: e.scalar_tensor_tensor(out=rankp[:, j, :], in0=ps[:, 0:E], scalar=1.0,
                                                          in1=k.rtM[:, j, :], op0=ALU.add, op1=ALU.mult),
             reads=[("pf", 4), ("rtM", j)], writes=[("rankp", j)])
        P.op("dve", lambda e, j=j: e.tensor_scalar(out=rankp[:, j, :], in0=rankp[:, j, :], scalar1=-1.0, scalar2=None,
                                                   op0=ALU.add), reads=[("rankp", j)], writes=[("rankp", j)])
    h1res = [("h1T", i) for i in range(NT)]

    def shared_hidden(ft, tg, it):
        bg, bu = (0, 1) if it % 2 == 0 else (2, 3)
        G, U = k.pf[bg], k.pf[bu]
        ts_ = slice(tg * 512, (tg + 1) * 512)
        fs = slice(ft * 128, (ft + 1) * 128)
        for a in range(16):
            P.op("pe", lambda e, a=a: e.matmul(G[:, :], wsg[:, a, fs], k.h1T[:, a, ts_], start=(a == 0), stop=(a == 15)),
                 reads=["wsg"] + h1res, writes=[("pf", bg)])
        for a in range(16):
            P.op("pe", lambda e, a=a: e.matmul(U[:, :], wsu[:, a, fs], k.h1T[:, a, ts_], start=(a == 0), stop=(a == 15)),
                 reads=["wsu"] + h1res, writes=[("pf", bu)])
        sg = sgs[it % 2]
        P.op("act", lambda e: e.activation(out=sg[:], in_=G[:, :], func=AF.Silu), reads=[("pf", bg)],
             writes=[("sgs", it % 2)])
        P.op("dve", lambda e: e.tensor_tensor(out=k.hsT[:, ft, ts_], in0=sg[:], in1=U[:, :], op=ALU.mult),
             reads=[("sgs", it % 2), ("pf", bu)], writes=["hsT"])

    sh_list = [(ft, tg) for ft in range(4) for tg in range(2)]
    sh_i = 0
    tw = k.pf[5]
    for ex in range(E):
        for i in range(NT):
            Sx = Sb[(ex % 2) * NT + i]
            P.op("dve", lambda e, Sx=Sx, i=i, ex=ex: e.tensor_scalar(out=Sx[:], in0=iota,
                                                                      scalar1=rankp[:, i, ex:ex + 1], scalar2=None,
                                                                      op0=ALU.is_equal),
                 reads=["cp", ("rankp", i)], writes=[("Sb", (ex % 2) * NT + i)])
        tw = k.pf[4 + ex % 2]
        twres = ("pf", 4 + ex % 2)
        for sb in range(2):
            c0 = (ex * 2 + sb) * 3
            for i in range(NT):
                Sx = Sb[(ex % 2) * NT + i]
                P.op("pe", lambda e, Sx=Sx, i=i, ex=ex, sb=sb, c0=c0, tw=tw: e.matmul(
                    tw[:, c0:c0 + 3], Sx[:, sb * 128:(sb + 1) * 128], VW[:, i, ex, :], start=(i == 0),
                    stop=(i == NT - 1)), reads=[("Sb", (ex % 2) * NT + i), ("VW0", i), ("VW1", i), ("VW2", i)],
                    writes=[twres])
        twv = tw[:, ex * 6:(ex + 1) * 6].rearrange("p (s c) -> p s c", c=3)
        P.op("dve", lambda e, twv=twv: e.tensor_scalar(out=padv[:], in0=twv[:, :, 2], scalar1=-1.0, scalar2=1.0,
                                                       op0=ALU.mult, op1=ALU.add), reads=[twres], writes=["padv"])
        P.op("dve", lambda e: e.tensor_tensor(out=padv[:], in0=padv[:], in1=k.cp[:, CP_SLOT:CP_SLOT + 2], op=ALU.mult),
             reads=["padv", "cp2"], writes=["padv"])
        P.op("dve", lambda e, twv=twv: e.scalar_tensor_tensor(out=padv[:], in0=twv[:, :, 0], scalar=float(TOK),
                                                              in1=padv[:], op0=ALU.add, op1=ALU.add),
             reads=[twres, "padv"], writes=["padv"])
        P.op("dve", lambda e, ex=ex: e.tensor_copy(out=k.idx[:, ex, :], in_=padv[:]), reads=["padv"],
             writes=[("idx", ex)])
        P.op("act", lambda e, ex=ex, twv=twv: e.copy(out=k.wsl[:, ex, :], in_=twv[:, :, 1]), reads=[twres],
             writes=[("wsl", ex)])
        if ex % 8 == 7 and sh_i < len(sh_list):
            shared_hidden(*sh_list[sh_i], sh_i)
            sh_i += 1
    while sh_i < len(sh_list):
        shared_hidden(*sh_list[sh_i], sh_i)
        sh_i += 1
    if k.dbg:
        P.dma("sp", k.dbgout["d_rt"][:, :, 0, :], k.rtM[:], reads=[("rtM", i) for i in range(NT)],
              writes=[("OUT", "rt0")])
        P.dma("sp", k.dbgout["d_rt"][:, :, 1, :], k.rtW[:], reads=[("rtW", i) for i in range(NT)],
              writes=[("OUT", "rt1")])
        P.dma("sp", k.dbgout["d_rt"][:, :, 2, :], rankp[:], reads=[("rankp", i) for i in range(NT)],
              writes=[("OUT", "rt2")])
        P.dma("sp", k.dbgout["d_idx"][:], k.idx[:], reads=[("idx", ex) for ex in range(E)], writes=[("OUT", "idx")])
    P.barrier()
    A.release("padv", "Mbf", "rankp", "VW", *[f"Sb{i}" for i in range(2 * NT)], "wsg", "wsu", "sgs0", "sgs1", "h1T", "rtM", "rtW")


def stage7b(k):
    P, A = k.P, k.A
    EL = getattr(k, "EL", E)
    wg = [A.alloc(f"wg{i}", [128, 16, FF], BF16) for i in range(2)]
    wu = [A.alloc(f"wu{i}", [128, 16, FF], BF16) for i in range(2)]
    wd = [A.alloc(f"wd{i}", [128, 4, D], BF16) for i in range(2)]
    xe = [A.alloc(f"xe{i}", [128, 2, D], BF16) for i in range(2)]
    xeT = [A.alloc(f"xeT{i}", [128, 16, CAP], BF16) for i in range(2)]
    hid = [A.alloc(f"hid{i}", [128, 4, CAP], BF16) for i in range(2)]
    sgt = [A.alloc(f"sgt{i}", [128, CAP], F32) for i in range(2)]
    yst = [A.alloc(f"yst{i}", [128, D], F32) for i in range(4)]
    h1bres = [("H1B", i) for i in range(NT)] + [("H1BZ", z) for z in range(CAP // 128)]
    accires = [("ACCI", i) for i in range(NT)]

    def load_w(ex):
        sl = ex % 2
        cast_load_rows(k, wg[sl], k.w_eg[ex], 16, FF, ("wg", sl))
        cast_load_rows(k, wu[sl], k.w_eu[ex], 16, FF, ("wu", sl))
        cast_load_rows(k, wd[sl], k.w_ed[ex], 4, D, ("wd", sl))

    def gather(ex):
        sl = ex % 2
        for sb in range(2):
            P.op("pool", lambda e, ex=ex, sb=sb, sl=sl: e.indirect_dma_start(
                out=xe[sl][:, sb, :], out_offset=None, in_=k.h1b[:, :],
                in_offset=bass.IndirectOffsetOnAxis(ap=k.idx[:, ex, sb:sb + 1], axis=0)), reads=[("idx", ex)] + h1bres, writes=[("xe", sl, sb)], dma=True)

    load_w(0)
    gather(0)
    evn = 0
    MODE = getattr(k, "S7MODE", 3)
    for ex in range(EL):
        if ex + 1 < EL:
            load_w(ex + 1)
            gather(ex + 1)
        sl = ex % 2
        if MODE == 1:
            P.op("dve", lambda e: e.memset(k.cst[:, 6:7], 0.0), reads=[("wg", sl), ("wu", sl), ("wd", sl), ("xe", sl, 0),
                                                                      ("xe", sl, 1)], writes=["cst6"])
            continue
        for sb in range(2):
            for hf in range(2):
                pbi = (sb * 2 + hf) % 2
                pbk = k.pb[pbi]
                for a8 in range(8):
                    a = hf * 8 + a8
                    P.op("pe", lambda e, a=a, a8=a8, sb=sb, sl=sl, pbk=pbk: e.transpose(
                        pbk[:, a8 * 128:(a8 + 1) * 128], xe[sl][:, sb, a * 128:(a + 1) * 128], k.ident_bf[:]),
                        reads=[("xe", sl, sb), "ident_bf"], writes=[("pb", pbi)])
                evac(k, "act", xeT[sl][:, hf * 8:(hf + 1) * 8, sb * 128:(sb + 1) * 128],
                     pbk[:, :].rearrange("p (a t) -> p a t", a=8), [("pb", pbi)], [("xeT", sl, sb, hf)])
        xres = [("xeT", sl, sb, hf) for sb in range(2) for hf in range(2)]
        if MODE == 4:
            P.op("dve", lambda e: e.memset(k.cst[:, 6:7], 0.0), reads=xres + [("wg", sl), ("wu", sl), ("wd", sl)],
                 writes=["cst6"])
            continue
        for ft in range(4):
            bg, bu = (0, 1) if ft % 2 == 0 else (2, 3)
            G, U = k.pf[bg], k.pf[bu]
            fs = slice(ft * 128, (ft + 1) * 128)
            for a in range(16):
                P.op("pe", lambda e, a=a, G=G, fs=fs, sl=sl: e.matmul(
                    G[:, 0:CAP], wg[sl][:, a, fs], xeT[sl][:, a, :], start=(a == 0), stop=(a == 15)),
                    reads=[("wg", sl)] + xres, writes=[("pf", bg)])
            for a in range(16):
                P.op("pe", lambda e, a=a, U=U, fs=fs, sl=sl: e.matmul(
                    U[:, 0:CAP], wu[sl][:, a, fs], xeT[sl][:, a, :], start=(a == 0), stop=(a == 15)),
                    reads=[("wu", sl)] + xres, writes=[("pf", bu)])
            sg = sgt[ft % 2]
            P.op("act", lambda e, G=G, sg=sg: e.activation(out=sg[:], in_=G[:, 0:CAP], func=AF.Silu),
                 reads=[("pf", bg)], writes=[("sgt", ft % 2)])
            P.op("dve", lambda e, U=U, sg=sg, ft=ft, sl=sl: e.tensor_tensor(
                out=hid[sl][:, ft, :], in0=sg[:], in1=U[:, 0:CAP], op=ALU.mult),
                reads=[("sgt", ft % 2), ("pf", bu)], writes=[("hid", sl, ft)])
        hres = [("hid", sl, ft) for ft in range(4)]
        if MODE == 5:
            P.op("dve", lambda e: e.memset(k.cst[:, 6:7], 0.0), reads=hres + [("wd", sl)], writes=["cst6"])
            continue
        for sb in range(2):
            yb = (ex % 2) * 2 + sb
            y = yst[yb]
            for dg in range(4):
                b = 4 + evn % 2
                evn += 1
                bank = k.pf[b]
                dsl = slice(dg * 512, (dg + 1) * 512)
                for ft in range(4):
                    P.op("pe", lambda e, ft=ft, bank=bank, sb=sb, sl=sl, dsl=dsl: e.matmul(
                        bank[:, :], hid[sl][:, ft, sb * 128:(sb + 1) * 128], wd[sl][:, ft, dsl], start=(ft == 0),
                        stop=(ft == 3)), reads=hres + [("wd", sl)], writes=[("pf", b)])
                P.op("dve", lambda e, bank=bank, y=y, dsl=dsl, ex=ex, sb=sb: e.tensor_scalar(
                    out=y[:, dsl], in0=bank[:, :], scalar1=k.wsl[:, ex, sb:sb + 1], scalar2=None, op0=ALU.mult),
                    reads=[("pf", b), ("wsl", ex)], writes=[("yst", yb)])
            if MODE == 2:
                continue
            P.op("pool", lambda e, ex=ex, sb=sb, y=y: e.indirect_dma_start(
                out=k.acc[:, :], out_offset=bass.IndirectOffsetOnAxis(ap=k.idx[:, ex, sb:sb + 1], axis=0),
                in_=y[:, :], in_offset=None, compute_op=ALU.add),
                reads=[("yst", yb), ("idx", ex)] + accires + ([("ACCW", ex - 1, 0), ("ACCW", ex - 1, 1)] if ex else []),
                writes=[("ACCW", ex, sb)], dma=True)
    P.op("dve", lambda e: e.memset(k.cst[:, 6:7], 0.0), reads=[("ACCW", EL - 1, 0), ("ACCW", EL - 1, 1)],
         writes=["ACC"])
    P.barrier()
    A.release("wg0", "wg1", "wu0", "wu1", "wd0", "wd1", "xe0", "xe1", "xeT0", "xeT1", "hid0", "hid1", "sgt0", "sgt1",
              "yst0", "yst1", "yst2", "yst3", "idx", "wsl")


def stage8(k):
    P, A = k.P, k.A
    wsd = A.alloc("wsd", [128, 4, D], BF16)
    lng = A.alloc("lng2", [128, 2, D], F32)
    ats = [A.alloc(f"at{i}", [128, D], F32) for i in range(2)]
    lnst = A.alloc("lnst2", [128, 4, 6], F32)
    mv = A.alloc("mv2", [128, 8], F32)
    cast_load_rows(k, wsd, k.w_sd, 4, D, "wsd")
    P.dma("sp", lng[:], k.lnp[:, 2:4, :], writes=["lng2"])
    for i in range(NT):
        at = ats[i % 2]
        ares = ("at", i % 2)
        tsl = slice(i * 128, (i + 1) * 128)
        P.dma("sp", at[:], k.acc[tsl, :], reads=["ACC", ("ACCI", i)], writes=[ares])
        for dg in range(4):
            b = dg % 2
            ps = k.pf[b]
            dsl = slice(dg * 512, (dg + 1) * 512)
            for ft in range(4):
                P.op("pe", lambda e, ft=ft, ps=ps, tsl=tsl, dsl=dsl: e.matmul(
                    ps[:, :], k.hsT[:, ft, tsl], wsd[:, ft, dsl], start=(ft == 0), stop=(ft == 3)),
                    reads=["hsT", "wsd"], writes=[("pf", b)])
            P.op("dve", lambda e, ps=ps, at=at, dsl=dsl: e.tensor_tensor(out=at[:, dsl], in0=at[:, dsl], in1=ps[:, :],
                                                                        op=ALU.add), reads=[ares, ("pf", b)],
                 writes=[ares])
            P.op("dve", lambda e, at=at, dsl=dsl, dg=dg: e.bn_stats(out=lnst[:, dg, :], in_=at[:, dsl]), reads=[ares],
                 writes=[("ln2", "st", dg)])
        layer_norm_tile(k, at, ares, lng, "lng2", lnst, mv, "ln2")
        P.dma("sp", k.out[tsl, :], at[:], reads=[ares], writes=[("OUT", "out", i)])


def _t5_bucket_np():
    import jax
    import jax.numpy as jnp
    with jax.default_device(jax.devices("cpu")[0]):
        qi = jnp.arange(128)[:, None]
        c = jnp.arange(384)[None, :]
        rel = c - 128 - qi
        nb = 16
        ret = jnp.where(rel > 0, nb, 0)
        n = jnp.abs(rel)
        max_exact = nb // 2
        nf = jnp.maximum(n, 1).astype(jnp.float32)
        large = max_exact + (jnp.log(nf / max_exact) / math.log(128 / max_exact) * (nb - max_exact)).astype(jnp.int32)
        large = jnp.minimum(large, nb - 1)
        bucket = np.asarray(ret + jnp.where(n < max_exact, n, large))
        rel = np.asarray(rel)
    return bucket, rel


def _rope_tables():
    import jax
    import jax.numpy as jnp
    with jax.default_device(jax.devices("cpu")[0]):
        rows = S // 64
        row = jnp.broadcast_to(jnp.arange(rows)[:, None], (rows, 64)).reshape(S).astype(jnp.float32)
        col = jnp.broadcast_to(jnp.arange(64)[None, :], (rows, 64)).reshape(S).astype(jnp.float32)
        half = DH // 2
        inv = 10000.0 ** (-jnp.arange(0, half, 2, dtype=jnp.float32) / half)
        ang_r = row[:, None] * inv
        ang_c = col[:, None] * inv
        cr, sr, cc, sc = (np.asarray(t) for t in (jnp.cos(ang_r), jnp.sin(ang_r), jnp.cos(ang_c), jnp.sin(ang_c)))
    Ct = np.concatenate([cr, cr, cc, cc], axis=1)
    St = np.concatenate([-sr, sr, -sc, sc], axis=1)
    return np.ascontiguousarray(np.concatenate([Ct, St], axis=1).astype(np.float32))


def _const_pack():
    cp = np.zeros((128, CP_N), np.float32)
    cp[:, CP_IDENT:CP_IDENT + 128] = np.eye(128, dtype=np.float32)
    cp[:, CP_IOTA:CP_IOTA + 256] = np.arange(256, dtype=np.float32)[None, :]
    cp[:, CP_U:CP_U + 128] = np.triu(np.ones((128, 128), np.float32), 1)
    bucket, rel = _t5_bucket_np()
    valid = np.abs(rel) <= 128
    cp[:, CP_IDXA:CP_IDXA + 384] = np.where(valid, bucket, -1).astype(np.float32)
    cp[:, CP_MASKA:CP_MASKA + 384] = np.where(valid, 0.0, NEG).astype(np.float32)
    for i in range(NT):
        cp[:, CP_VCOL + i] = np.arange(128, dtype=np.float32) + i * 128 - TOK
    cp[:, CP_SLOT] = np.arange(128, dtype=np.float32)
    cp[:, CP_SLOT + 1] = np.arange(128, dtype=np.float32) + 128
    return cp


def make_in_maps(inputs):
    f = lambda a: np.ascontiguousarray(np.asarray(a, dtype=np.float32))
    x = f(inputs["x"]).reshape(S, D)
    xT = np.ascontiguousarray(x.T)
    xTpad = np.zeros((D, S + 2 * HALO), np.float32)
    xTpad[:, HALO:HALO + S] = xT
    ropeK = _rope_tables()
    cp = _const_pack()
    shared = dict(
        xT=xT, w_in=f(inputs["w_in"]).reshape(D, IN_COLS), w_branch_a=f(inputs["w_branch_a"]).reshape(NH * DH, D),
        w_branch_b=f(inputs["w_branch_b"]).reshape(NH * DH, D), w_out=f(inputs["w_out"]).reshape(D, D),
        w_router=f(inputs["w_router"]).reshape(D, E), w_exp_gate=f(inputs["w_exp_gate"]).reshape(E, D, FF),
        w_exp_up=f(inputs["w_exp_up"]).reshape(E, D, FF), w_exp_down=f(inputs["w_exp_down"]).reshape(E, FF, D),
        w_sh_gate=f(inputs["w_sh_gate"]).reshape(D, FF), w_sh_up=f(inputs["w_sh_up"]).reshape(D, FF),
        w_sh_down=f(inputs["w_sh_down"]).reshape(FF, D), cpack=cp, ropeK=ropeK)
    lnp = np.stack([f(inputs[n]).reshape(D) for n in ("ln1_g", "ln1_b", "ln2_g", "ln2_b")])
    shared["lnp"] = np.ascontiguousarray(np.broadcast_to(lnp[None], (128, 4, D)))
    pp = np.zeros((128, PP_N), np.float32)
    pp[:, PP_SINK:PP_SINK + 8] = f(inputs["attn_sink"]).reshape(1, 8)
    pp[:, PP_TAB:PP_TAB + 256] = f(inputs["rel_bias_table"]).reshape(1, 256)
    pp[:, PP_GQ:PP_GQ + 128] = f(inputs["q_norm_g"]).reshape(1, 128)
    pp[:, PP_GK:PP_GK + 128] = f(inputs["k_norm_g"]).reshape(1, 128)
    pp[:, PP_RB:PP_RB + 64] = f(inputs["router_bias"]).reshape(1, 64)
    pp[:, PP_BG:PP_BG + 32] = f(inputs["b_gate"]).reshape(2, 16, 128).transpose(2, 0, 1).reshape(128, 32)
    maps = []
    for c in range(NCORES):
        m = dict(shared)
        m["xTh"] = np.ascontiguousarray(xTpad[:, c * TOK:c * TOK + TH])
        m["x_own"] = np.ascontiguousarray(x[c * TOK:(c + 1) * TOK])
        m["ropeQ"] = np.ascontiguousarray(ropeK[c * TOK:(c + 1) * TOK])
        ppc = pp.copy()
        ppc[:, PP_EDGE] = NEG if c == 0 else 0.0
        ppc[:, PP_EDGE + 1] = NEG if c == NCORES - 1 else 0.0
        m["ppack"] = ppc
        maps.append(m)
    return maps


_CACHE = {}


def kernel(**inputs):
    if "nc" not in _CACHE:
        _CACHE["nc"] = build_program()[0]
    nc = _CACHE["nc"]
    maps = make_in_maps(inputs)
    res = run_bass_kernel_spmd(nc, maps, core_ids=list(range(NCORES)))
    out = np.concatenate([np.asarray(r["out"], dtype=np.float32) for r in res.results], axis=0)
    return out.reshape(1, S, D)
```

```python
import math
from contextlib import ExitStack

import numpy as np
import concourse.bass as bass
import concourse.mybir as mybir
from concourse.bass_utils import run_bass_kernel_spmd

F32 = mybir.dt.float32
BF16 = mybir.dt.bfloat16
I32 = mybir.dt.int32
U8 = mybir.dt.uint8
AF = mybir.ActivationFunctionType
ALU = mybir.AluOpType
AX = mybir.AxisListType

NCORES = 8
S = 8192
D = 2048
TOK = S // NCORES
NT = TOK // 128
HALO = 128
TH = TOK + 2 * HALO
DH = 128
NH = 8
NKV = 2
E = 64
FF = 512
CAP = 256
IN_COLS = 7168
C_QA, C_KA, C_VA, C_QB, C_KB, C_VB, C_GA, C_GB = 0, 1024, 1280, 1536, 2560, 2816, 3072, 5120
ALPHA = 2.0 ** 0.25
SCALE = DH ** -0.5
NEG = -30000.0
BIG = 1.0e9

NDSEM = 8


class Op:
    __slots__ = ("eng", "fn", "dma", "deps", "sig", "sval", "dsem", "dval")

    def __init__(self, eng, fn, dma):
        self.eng = eng
        self.fn = fn
        self.dma = dma
        self.deps = ()
        self.sig = False
        self.sval = 0
        self.dsem = None
        self.dval = 0


class Prog:
    ENGS = ("pe", "act", "dve", "pool", "sp")

    def __init__(self, nc, stack):
        self.nc = nc
        self.stack = stack
        self.ops = []
        self.last_w = {}
        self.readers = {}
        self.last_c = {}
        self.dq = {"sp": [], "pool": [], "act": []}
        self.regs = {}

    def op(self, eng, fn, reads=(), writes=(), dma=False, extra=(), pwrites=()):
        idx = len(self.ops)
        o = Op(eng, fn, dma)
        deps = set(extra)
        for r in reads:
            deps.update(self.last_w.get(r, ()))
        for w in writes:
            deps.update(self.last_w.get(w, ()))
            rd = self.readers.get(w)
            if rd:
                deps.update(rd[0].values())
                deps.update(rd[1])
        for w in pwrites:
            rd = self.readers.get(w)
            if rd and (rd[0] or rd[1]):
                deps.update(rd[0].values())
                deps.update(rd[1])
                self.last_w[w] = []
                self.readers[w] = ({}, [])
        for r in reads:
            rd = self.readers.get(r)
            if rd is None:
                rd = self.readers[r] = ({}, [])
            if dma:
                rd[1].append(idx)
            else:
                rd[0][eng] = idx
        for w in writes:
            self.last_w[w] = [idx]
            self.readers[w] = ({}, [])
        for w in pwrites:
            self.last_w.setdefault(w, []).append(idx)
        deps.discard(idx)
        o.deps = tuple(deps)
        self.ops.append(o)
        if dma:
            self.dq[eng].append(idx)
        elif fn is not None:
            self.last_c[eng] = idx
        return idx

    def dma(self, eng, out, in_, reads=(), writes=(), pwrites=(), **kw):
        return self.op(eng, lambda e: e.dma_start(out=out, in_=in_, **kw), reads, writes, dma=True, pwrites=pwrites)

    def reg(self, e, val):
        if val not in self.regs:
            self.regs[val] = e.to_reg(val)
        return self.regs[val]

    def barrier(self):
        deps = set(self.last_c.values())
        for q in self.dq.values():
            deps.update(q[-NDSEM:])
        for e in self.ENGS:
            self.op(e, None, extra=tuple(deps))

    def finalize(self):
        nc = self.nc
        ops = self.ops
        for o in ops:
            for d in o.deps:
                p = ops[d]
                if p.eng == "pe" and o.eng == "pe" and not p.dma and not o.dma:
                    continue
                p.sig = True
        csem = {e: self.stack.enter_context(nc.semaphore(f"c_{e}")) for e in self.ENGS}
        dsems = {e: [self.stack.enter_context(nc.semaphore(f"d_{e}{i}")) for i in range(NDSEM)]
                 for e in ("sp", "pool", "act")}
        cnt = {e: 0 for e in self.ENGS}
        dcnt = {e: 0 for e in dsems}
        for o in ops:
            if o.dma:
                j = dcnt[o.eng]
                dcnt[o.eng] += 1
                o.dsem = (o.eng, j % NDSEM)
                o.dval = 16 * (j // NDSEM + 1)
            elif o.sig:
                assert o.fn is not None
                cnt[o.eng] += 1
                o.sval = cnt[o.eng]
        self.stats = dict(cnt=cnt, dcnt=dcnt, nops=len(ops))

        def run(engname, e):
            waited = {}
            for o in ops:
                if o.eng != engname:
                    continue
                need = {}
                for d in o.deps:
                    p = ops[d]
                    if p.dma:
                        key = ("d",) + p.dsem
                        val = p.dval
                    else:
                        if p.eng == "pe" and engname == "pe" and not o.dma:
                            continue
                        key = ("c", p.eng)
                        val = p.sval
                    if val > need.get(key, 0):
                        need[key] = val
                if o.dma and o.dval > 16:
                    key = ("d",) + o.dsem
                    need[key] = max(need.get(key, 0), o.dval - 16)
                for key, val in need.items():
                    if val > waited.get(key, 0):
                        sem = csem[key[1]] if key[0] == "c" else dsems[key[1]][key[2]]
                        e.wait_ge(sem, val)
                        waited[key] = val
                if o.fn is None:
                    continue
                ins = o.fn(e)
                if o.dma:
                    ins.then_inc(dsems[o.dsem[0]][o.dsem[1]], 16)
                elif o.sig:
                    ins.then_inc(csem[engname], 1)

        with nc.Block() as block:
            @block.tensor
            def _(e):
                run("pe", e)

            @block.scalar
            def _(e):
                run("act", e)

            @block.vector
            def _(e):
                run("dve", e)

            @block.gpsimd
            def _(e):
                run("pool", e)

            @block.sync
            def _(e):
                run("sp", e)


_DTSZ = {F32: 4, BF16: 2, I32: 4, U8: 1}


class Arena:
    def __init__(self, raw, nbytes):
        self.raw = raw
        self.free = [(0, nbytes)]
        self.live = {}
        self.peak = 0

    def alloc(self, name, shape, dt):
        n = 1
        for s in shape[1:]:
            n *= s
        nb = (n * _DTSZ[dt] + 63) // 64 * 64
        for i, (off, sz) in enumerate(self.free):
            if sz >= nb:
                if sz == nb:
                    self.free.pop(i)
                else:
                    self.free[i] = (off + nb, sz - nb)
                break
        else:
            raise MemoryError(f"arena: cannot alloc {name} {nb}B; free={self.free}")
        self.live[name] = (off, nb)
        self.peak = max(self.peak, off + nb)
        ap = self.raw[:, off:off + n * _DTSZ[dt]].bitcast(dt)
        if len(shape) == 3:
            ap = ap.rearrange("p (a b) -> p a b", a=shape[1])
        elif len(shape) == 4:
            ap = ap.rearrange("p (a b c) -> p a b c", a=shape[1], b=shape[2])
        return ap

    def release(self, *names):
        for name in names:
            off, nb = self.live.pop(name)
            self.free.append((off, nb))
        self.free.sort()
        merged = []
        for off, sz in self.free:
            if merged and merged[-1][0] + merged[-1][1] == off:
                merged[-1] = (merged[-1][0], merged[-1][1] + sz)
            else:
                merged.append((off, sz))
        self.free = merged


CP_IDENT, CP_IOTA, CP_U, CP_IDXA, CP_MASKA, CP_VCOL, CP_SLOT, CP_N = 0, 128, 384, 512, 896, 1280, 1296, 1298
PP_SINK, PP_TAB, PP_GQ, PP_GK, PP_RB, PP_BG, PP_EDGE, PP_N = 0, 8, 264, 392, 520, 584, 616, 618


class K:
    pass


def build_program(stop_after=99, dbg=False):
    nc = bass.Bass("TRN2", target_bir_lowering=False)
    k = K()
    k.nc = nc
    k.dbg = dbg
    k.stop_after = stop_after

    k.in_names = []

    def din(name, shape, dt=F32):
        need = {"xT": 3, "x_own": 6, "w_branch_a": 5, "w_branch_b": 5, "w_out": 6, "w_router": 6, "w_exp_gate": 8,
                "w_exp_up": 8, "w_exp_down": 8, "w_sh_gate": 7, "w_sh_up": 7, "w_sh_down": 9, "lnp": 6, "ropeK": 3}
        if stop_after < need.get(name, 0):
            shape = [1] * len(shape)
        k.in_names.append((name, tuple(shape)))
        return nc.dram_tensor(name, list(shape), dt, kind="ExternalInput").ap()

    k.xTh = din("xTh", [D, TH])
    k.xT = din("xT", [D, S])
    k.x_own = din("x_own", [TOK, D])
    k.w_in = din("w_in", [D, IN_COLS])
    k.w_a = din("w_branch_a", [NH * DH, D])
    k.w_b = din("w_branch_b", [NH * DH, D])
    k.w_out = din("w_out", [D, D])
    k.w_router = din("w_router", [D, E])
    k.w_eg = din("w_exp_gate", [E, D, FF])
    k.w_eu = din("w_exp_up", [E, D, FF])
    k.w_ed = din("w_exp_down", [E, FF, D])
    k.w_sg = din("w_sh_gate", [D, FF])
    k.w_su = din("w_sh_up", [D, FF])
    k.w_sd = din("w_sh_down", [FF, D])
    k.cpack = din("cpack", [128, CP_N])
    k.ppack = din("ppack", [128, PP_N])
    k.lnp = din("lnp", [128, 4, D])
    k.ropeK = din("ropeK", [S, 256])
    k.ropeQ = din("ropeQ", [TOK, 256])
    k.out = nc.dram_tensor("out", [TOK, D], F32, kind="ExternalOutput").ap()
    k.h1b = nc.dram_tensor("h1b_scr", [TOK + CAP, D], BF16).ap()
    k.acc = nc.dram_tensor("acc_scr", [TOK + CAP, D], F32).ap()
    k.dbgout = {}
    if dbg:
        for nm, shp, dt in (("d_oaT", [128, NH, TOK], BF16), ("d_obT", [128, NH, TOK], BF16),
                            ("d_mT", [128, 16, TOK], BF16), ("d_h1", [TOK, D], F32),
                            ("d_rt", [128, NT, 3, E], F32), ("d_idx", [128, E, 2], I32)):
            k.dbgout[nm] = nc.dram_tensor(nm, shp, dt, kind="ExternalOutput").ap()

    with ExitStack() as st:
        P = Prog(nc, st)
        k.P = P
        ARENA_BYTES = 207 * 1024
        raw = st.enter_context(nc.sbuf_tensor("arena", [128, ARENA_BYTES], U8))
        k.A = Arena(raw, ARENA_BYTES)
        k.pf = [st.enter_context(nc.psum_tensor(f"pf{i}", [128, 512], F32)) for i in range(6)]
        k.pb = [st.enter_context(nc.psum_tensor(f"pb{i}", [128, 1024], BF16)) for i in range(2)]
        stages = [stage0, stage1, stage2, stage3, stage4, stage5, stage6, stage7, stage7b, stage8]
        for i, fn in enumerate(stages):
            if i > stop_after:
                break
            fn(k)
        outs = [r for r in P.last_w if isinstance(r, tuple) and r[0] == "OUT"]
        P.op("sp", None, reads=outs)
        P.finalize()
        k.stats = P.stats
        k.peak = k.A.peak
    return nc, k


def cast_load_rows(k, dst3, src2, nk, ncols, res, k0=0):
    P = k.P
    srcv = src2.rearrange("(a p) c -> p a c", p=128)
    if ncols >= 1024:
        assert ncols % 1024 == 0
        for a in range(nk):
            for c0 in range(0, ncols, 1024):
                P.dma("pool", dst3[:, a, c0:c0 + 1024], srcv[:, k0 + a, c0:c0 + 1024], pwrites=[res])
    else:
        per = 1024 // ncols
        for a in range(0, nk, per):
            n = min(per, nk - a)
            P.dma("pool", dst3[:, a:a + n, :], srcv[:, k0 + a:k0 + a + n, :], pwrites=[res])


def evac(k, which, out, in_, reads, writes):
    P = k.P
    if which == "act":
        P.op("act", lambda e: e.copy(out=out, in_=in_), reads, writes)
    else:
        P.op("dve", lambda e: e.tensor_copy(out=out, in_=in_), reads, writes)


def stage0(k):
    P, A = k.P, k.A
    k.cp = A.alloc("cp", [128, CP_N], F32)
    k.pp = A.alloc("pp", [128, PP_N], F32)
    k.cst = A.alloc("cst", [128, 8], F32)
    k.ident_bf = A.alloc("ident_bf", [128, 128], BF16)
    k.ones_bf = A.alloc("ones_bf", [128, 128], BF16)
    k.u_bf = A.alloc("u_bf", [128, 128], BF16)
    P.dma("sp", k.cp[:, 0:640], k.cpack[:, 0:640], writes=["cp"])
    P.dma("sp", k.cp[:, 640:CP_N], k.cpack[:, 640:CP_N], writes=["cp2"])
    P.dma("sp", k.pp[:], k.ppack[:], writes=["pp"])
    P.op("dve", lambda e: e.memset(k.cst[:, 0:1], 0.0), writes=["cst"])
    P.op("dve", lambda e: e.memset(k.cst[:, 1:2], 1e-6), writes=["cst1"])
    P.op("dve", lambda e: e.memset(k.cst[:, 2:3], 1e-5), writes=["cst2"])
    P.op("dve", lambda e: e.memset(k.ones_bf[:], 1.0), writes=["ones_bf"])
    P.op("dve", lambda e: e.tensor_copy(out=k.ident_bf[:], in_=k.cp[:, CP_IDENT:CP_IDENT + 128]),
         reads=["cp"], writes=["ident_bf"])
    P.op("dve", lambda e: e.tensor_copy(out=k.u_bf[:], in_=k.cp[:, CP_U:CP_U + 128]), reads=["cp"],
         writes=["u_bf"])
    P.op("dve", lambda e: e.tensor_reduce(out=k.cst[:, 3:4], in_=k.pp[:, PP_GQ:PP_GQ + 128], axis=AX.X,
                                          op=ALU.max, apply_absolute_value=True), reads=["pp"], writes=["cst3"])
    P.op("dve", lambda e: e.tensor_reduce(out=k.cst[:, 4:5], in_=k.pp[:, PP_GK:PP_GK + 128], axis=AX.X,
                                          op=ALU.max, apply_absolute_value=True), reads=["pp"], writes=["cst4"])
    P.op("dve", lambda e: e.tensor_scalar(out=k.cst[:, 5:6], in0=k.cst[:, 3:4], scalar1=k.cst[:, 4:5],
                                          scalar2=-math.sqrt(DH), op0=ALU.mult, op1=ALU.mult),
         reads=["cst3", "cst4"], writes=["cst5"])


def norm_rope(k, src_ps, nh, gcol, ct, st_, dst, src_res, dst_res, tag):
    P = k.P
    junk, ssq, t0, t1, t2 = k.nr_junk, k.nr_ssq, k.nr_t0, k.nr_t1, k.nr_t2
    G = k.pp[:, gcol:gcol + 128]
    for h in range(nh):
        P.op("act", lambda e, h=h: e.activation(out=junk[:], in_=src_ps[:, h * 128:(h + 1) * 128], func=AF.Square,
                                                accum_out=ssq[:, h:h + 1]),
             reads=[src_res], writes=["nr_junk", ("nr_ssq", h)])
    P.op("act", lambda e: e.activation(out=ssq[:, 8:8 + nh], in_=ssq[:, 0:nh], func=AF.Sqrt, scale=1.0 / DH,
                                       bias=k.cst[:, 1:2]),
         reads=[("nr_ssq", h) for h in range(nh)] + ["cst1"], writes=["nr_std"])
    P.op("dve", lambda e: e.reciprocal(out=ssq[:, 16:16 + nh], in_=ssq[:, 8:8 + nh]), reads=["nr_std"],
         writes=["nr_rstd"])
    for h in range(nh):
        sl = slice(h * 128, (h + 1) * 128)
        P.op("dve", lambda e, h=h, sl=sl: e.scalar_tensor_tensor(out=t0[:], in0=src_ps[:, sl],
                                                                  scalar=ssq[:, 16 + h:17 + h], in1=G,
                                                                  op0=ALU.mult, op1=ALU.mult),
             reads=[src_res, "nr_rstd", "pp"], writes=["nr_t0"])
        P.op("dve", lambda e: e.tensor_tensor(out=t1[:], in0=t0[:], in1=ct, op=ALU.mult),
             reads=["nr_t0", tag], writes=["nr_t1"])
        t0v = t0.rearrange("p (a b j) -> p a b j", a=2, b=2)
        t2v = t2.rearrange("p (a b j) -> p a b j", a=2, b=2)
        sv = st_.rearrange("p (a b j) -> p a b j", a=2, b=2)
        P.op("dve", lambda e: e.tensor_tensor(out=t2v[:, :, 0, :], in0=t0v[:, :, 1, :], in1=sv[:, :, 0, :],
                                              op=ALU.mult), reads=["nr_t0", tag], writes=["nr_t2a"])
        P.op("dve", lambda e: e.tensor_tensor(out=t2v[:, :, 1, :], in0=t0v[:, :, 0, :], in1=sv[:, :, 1, :],
                                              op=ALU.mult), reads=["nr_t0", tag], writes=["nr_t2b"])
        P.op("dve", lambda e, sl=sl: e.tensor_tensor(out=dst[:, sl], in0=t1[:], in1=t2[:], op=ALU.add),
             reads=["nr_t1", "nr_t2a", "nr_t2b"], writes=[dst_res])


def stage1(k):
    P, A = k.P, k.A
    k.QbT = A.alloc("QbT", [128, NH, TOK], BF16)
    k.oaT = A.alloc("oaT", [128, NH, TOK], BF16)
    k.QaT = A.alloc("QaT", [128, NH, TOK], BF16)
    k.KaT = A.alloc("KaT", [128, NKV, TH], BF16)
    k.Va = A.alloc("Va", [128, TH // 128, NKV * DH], BF16)
    xTh = A.alloc("xTh", [128, 16, TH], BF16)
    wblk = [A.alloc(f"wblk{i}", [128, 16, 512], BF16) for i in range(2)]
    ropeq = A.alloc("ropeq", [128, NT, 256], F32)
    k.nr_junk = A.alloc("nr_junk", [128, 128], F32)
    k.nr_ssq = A.alloc("nr_ssq", [128, 24], F32)
    k.nr_t0 = A.alloc("nr_t0", [128, 128], F32)
    k.nr_t1 = A.alloc("nr_t1", [128, 128], F32)
    k.nr_t2 = A.alloc("nr_t2", [128, 128], F32)
    qn = [A.alloc(f"qn{i}", [128, 512], BF16) for i in range(2)]

    k.biasA = biasA = A.alloc("biasA", [128, NH, 384], F32)
    bmask = A.alloc("bmask", [128, 384], F32)
    idxA = k.cp[:, CP_IDXA:CP_IDXA + 384]
    maskA = k.cp[:, CP_MASKA:CP_MASKA + 384]
    for h in range(NH):
        P.op("dve", lambda e, h=h: e.tensor_copy(out=biasA[:, h, :], in_=maskA), reads=["cp2"],
             writes=[("biasA", h)])
    for b in range(32):
        P.op("dve", lambda e, b=b: e.tensor_scalar(out=bmask[:], in0=idxA, scalar1=float(b), scalar2=None,
                                                   op0=ALU.is_equal), reads=["cp", "cp2"], writes=["bmask"])
        for h in range(NH):
            P.op("dve", lambda e, b=b, h=h: e.scalar_tensor_tensor(
                out=biasA[:, h, :], in0=bmask[:], scalar=k.pp[:, PP_TAB + b * 8 + h:PP_TAB + b * 8 + h + 1],
                in1=biasA[:, h, :], op0=ALU.mult, op1=ALU.add), reads=["bmask", "pp", ("biasA", h)],
                writes=[("biasA", h)])
    P.dma("sp", ropeq[:], k.ropeQ.rearrange("(j p) c -> p j c", p=128), writes=["ropeq"])
    xv = k.xTh.rearrange("(a p) c -> p a c", p=128)
    for a in range(16):
        for hf in range(2):
            P.dma("pool", xTh[:, a, hf * 640:(hf + 1) * 640], xv[:, a, hf * 640:(hf + 1) * 640],
                  pwrites=[("xTh", a)])
    xres = [("xTh", a) for a in range(16)]
    blocks = [("qa", C_QA), ("qa", C_QA + 512), ("kv", C_KA), ("qb", C_QB), ("qb", C_QB + 512)]

    def load_blk(bi):
        kind, c0 = blocks[bi]
        cast_load_rows(k, wblk[bi % 2], k.w_in[:, c0:c0 + 512], 16, 512, ("wblk", bi % 2))

    load_blk(0)
    pfi = [0]
    evi = [0]

    def nextpf():
        pfi[0] = (pfi[0] + 1) % 4
        return pfi[0]

    def nextev():
        evi[0] ^= 1
        return "act" if evi[0] else "dve"

    for bi, (kind, c0) in enumerate(blocks):
        if bi + 1 < len(blocks):
            load_blk(bi + 1)
        w = wblk[bi % 2]
        wr = ("wblk", bi % 2)
        if kind == "qa":
            hb = (c0 - C_QA) // 128
            for hh in range(4):
                for tg in range(2):
                    b = nextpf()
                    ps = k.pf[b]
                    for a in range(16):
                        P.op("pe", lambda e, a=a, hh=hh, tg=tg, ps=ps, w=w: e.matmul(
                            ps[:, :], w[:, a, hh * 128:(hh + 1) * 128],
                            xTh[:, a, HALO + tg * 512:HALO + (tg + 1) * 512], start=(a == 0), stop=(a == 15)),
                            reads=[wr, ("xTh", a)], writes=[("pf", b)])
                    evac(k, nextev(), k.QaT[:, hb + hh, tg * 512:(tg + 1) * 512], ps[:, :], [("pf", b)],
                         [("QaT", hb + hh)])
        elif kind == "kv":
            for kvh in range(NKV):
                for g0, g1 in ((0, 512), (512, 1024), (1024, TH)):
                    b = nextpf()
                    ps = k.pf[b]
                    for a in range(16):
                        P.op("pe", lambda e, a=a, kvh=kvh, g0=g0, g1=g1, ps=ps, w=w: e.matmul(
                            ps[:, 0:g1 - g0], w[:, a, kvh * 128:(kvh + 1) * 128], xTh[:, a, g0:g1],
                            start=(a == 0), stop=(a == 15)), reads=[wr, ("xTh", a)], writes=[("pf", b)])
                    evac(k, nextev(), k.KaT[:, kvh, g0:g1], ps[:, 0:g1 - g0], [("pf", b)], ["KaT"])
            for t in range(TH // 128):
                b = nextpf()
                ps = k.pf[b]
                for a in range(16):
                    P.op("pe", lambda e, a=a, t=t, ps=ps, w=w: e.matmul(
                        ps[:, 0:256], xTh[:, a, t * 128:(t + 1) * 128], w[:, a, 256:512],
                        start=(a == 0), stop=(a == 15)), reads=[wr, ("xTh", a)], writes=[("pf", b)])
                evac(k, nextev(), k.Va[:, t, :], ps[:, 0:256], [("pf", b)], ["Va"])
        else:
            hb = (c0 - C_QB) // 128
            for i in range(NT):
                b = nextpf()
                ps = k.pf[b]
                for a in range(16):
                    P.op("pe", lambda e, a=a, i=i, ps=ps, w=w: e.matmul(
                        ps[:, :], xTh[:, a, HALO + i * 128:HALO + (i + 1) * 128], w[:, a, :],
                        start=(a == 0), stop=(a == 15)), reads=[wr, ("xTh", a)], writes=[("pf", b)])
                q = qn[i % 2]
                qres = ("qn", i % 2)
                norm_rope(k, ps, 4, PP_GQ, ropeq[:, i, 0:128], ropeq[:, i, 128:256], q, ("pf", b), qres, "ropeq")
                pb = k.pb[i % 2]
                for hh in range(4):
                    P.op("pe", lambda e, hh=hh, pb=pb, q=q: e.transpose(pb[:, hh * 128:(hh + 1) * 128],
                                                                        q[:, hh * 128:(hh + 1) * 128],
                                                                        k.ident_bf[:]),
                         reads=[qres, "ident_bf"], writes=[("pb", i % 2)])
                evac(k, "act", k.QbT[:, hb:hb + 4, i * 128:(i + 1) * 128],
                     pb[:, 0:512].rearrange("p (h t) -> p h t", h=4), [("pb", i % 2)], ["QbT"])
    P.barrier()
    A.release("xTh", "wblk0", "wblk1", "ropeq", "qn0", "qn1")


def stage2(k):
    P, A = k.P, k.A
    biasA = k.biasA
    ssb = [A.alloc(f"ssb{i}", [128, 384], F32) for i in range(2)]
    pfp = [A.alloc(f"pfp{i}", [128, 384], F32) for i in range(2)]
    pnb = [A.alloc(f"pnb{i}", [128, 384], BF16) for i in range(2)]
    pTs = [A.alloc(f"pTs{i}", [128, 384], BF16) for i in range(2)]
    stt = A.alloc("stt", [128, 2, 8], F32)
    units = [(n, kvh, hh) for n in range(NT) for kvh in range(NKV) for hh in range(4)]

    def part_a(u):
        n, kvh, hh = units[u]
        h = kvh * 4 + hh
        sb_i = u % 2
        sps = k.pf[sb_i]
        s_sb, p_f = ssb[sb_i], pfp[sb_i]
        R = lambda nm: (nm, sb_i)
        sv = stt[:, sb_i, :]
        P.op("pe", lambda e: e.matmul(sps[:, 0:384], k.QaT[:, h, n * 128:(n + 1) * 128],
                                      k.KaT[:, kvh, n * 128:n * 128 + 384], start=True, stop=True),
             reads=[("QaT", h), "KaT"], writes=[("pf", sb_i)])
        P.op("dve", lambda e: e.scalar_tensor_tensor(out=s_sb[:], in0=sps[:, 0:384], scalar=SCALE, in1=biasA[:, h, :],
                                                     op0=ALU.mult, op1=ALU.add),
             reads=[("pf", sb_i), ("biasA", h)], writes=[R("ssb")])
        if n == 0:
            P.op("dve", lambda e: e.tensor_scalar(out=s_sb[:, 0:128], in0=s_sb[:, 0:128],
                                                  scalar1=k.pp[:, PP_EDGE:PP_EDGE + 1], scalar2=None, op0=ALU.add),
                 reads=[R("ssb"), "pp"], writes=[R("ssb")])
        if n == NT - 1:
            P.op("dve", lambda e: e.tensor_scalar(out=s_sb[:, 256:384], in0=s_sb[:, 256:384],
                                                  scalar1=k.pp[:, PP_EDGE + 1:PP_EDGE + 2], scalar2=None, op0=ALU.add),
                 reads=[R("ssb"), "pp"], writes=[R("ssb")])
        P.op("dve", lambda e: e.reduce_max(out=sv[:, 0:1], in_=s_sb[:], axis=AX.X), reads=[R("ssb")], writes=[R("st0")])
        P.op("dve", lambda e: e.tensor_scalar(out=sv[:, 1:2], in0=sv[:, 0:1],
                                              scalar1=k.pp[:, PP_SINK + h:PP_SINK + h + 1], scalar2=-1.0,
                                              op0=ALU.max, op1=ALU.mult), reads=[R("st0"), "pp"], writes=[R("st1")])
        P.op("act", lambda e: e.activation(out=p_f[:], in_=s_sb[:], func=AF.Exp, bias=sv[:, 1:2], scale=1.0,
                                           accum_out=sv[:, 2:3]),
             reads=[R("ssb"), R("st1")], writes=[R("pfp"), R("st2")])
        P.op("act", lambda e: e.activation(out=sv[:, 3:4], in_=k.pp[:, PP_SINK + h:PP_SINK + h + 1], func=AF.Exp,
                                           bias=sv[:, 1:2], scale=1.0), reads=[R("st1"), "pp"], writes=[R("st3")])

    def part_b(u):
        n, kvh, hh = units[u]
        ob = 4 + kvh
        sb_i = u % 2
        p_f, p_n, pT = pfp[sb_i], pnb[sb_i], pTs[sb_i]
        R = lambda nm: (nm, sb_i)
        sv = stt[:, sb_i, :]
        P.op("dve", lambda e: e.tensor_tensor(out=sv[:, 4:5], in0=sv[:, 2:3], in1=sv[:, 3:4], op=ALU.add),
             reads=[R("st2"), R("st3")], writes=[R("st4")])
        P.op("dve", lambda e: e.reciprocal(out=sv[:, 5:6], in_=sv[:, 4:5]), reads=[R("st4")], writes=[R("st5")])
        P.op("dve", lambda e: e.tensor_scalar(out=p_n[:], in0=p_f[:], scalar1=sv[:, 5:6], scalar2=None, op0=ALU.mult),
             reads=[R("pfp"), R("st5")], writes=[R("pnb")])
        pb = k.pb[sb_i]
        for j in range(3):
            P.op("pe", lambda e, j=j: e.transpose(pb[:, j * 128:(j + 1) * 128], p_n[:, j * 128:(j + 1) * 128],
                                                  k.ident_bf[:]),
                 reads=[R("pnb"), "ident_bf"], writes=[("pb", sb_i)])
        evac(k, "act", pT[:], pb[:, 0:384], [("pb", sb_i)], [R("pTs")])
        ops_ = k.pf[ob]
        for j in range(3):
            P.op("pe", lambda e, j=j: e.matmul(ops_[:, hh * 128:(hh + 1) * 128],
                                               k.Va[:, n + j, kvh * 128:(kvh + 1) * 128],
                                               pT[:, j * 128:(j + 1) * 128], start=(j == 0), stop=(j == 2)),
                 reads=["Va", R("pTs")], writes=[("pf", ob)])
        if hh == 3:
            evac(k, "dve", k.oaT[:, kvh * 4:(kvh + 1) * 4, n * 128:(n + 1) * 128],
                 k.pf[ob][:, :].rearrange("p (h t) -> p h t", h=4), [("pf", ob)], ["oaT"])

    part_a(0)
    for u in range(1, len(units)):
        part_a(u)
        part_b(u - 1)
    part_b(len(units) - 1)
    if k.dbg:
        P.dma("sp", k.dbgout["d_oaT"][:], k.oaT[:], reads=["oaT"], writes=[("OUT", "d_oaT")])
    P.barrier()
    A.release("biasA", "bmask", "ssb0", "ssb1", "pfp0", "pfp1", "pnb0", "pnb1", "pTs0", "pTs1", "stt",
              "QaT", "KaT", "Va")


def stage3(k):
    P, A = k.P, k.A
    k.obT = A.alloc("obT", [128, NH, TOK], BF16)
    k.KbT = A.alloc("KbT", [128, NKV, S], BF16)
    k.Vb = A.alloc("Vb", [128, S // 128, NKV * DH], BF16)
    wkv = A.alloc("wkv", [128, 16, 512], BF16)
    xc = [A.alloc(f"xc{i}", [128, 16, 512], BF16) for i in range(2)]
    rk = [A.alloc(f"rk{i}", [128, 4, 256], F32) for i in range(2)]
    kn = [A.alloc(f"kn{i}", [128, 256], BF16) for i in range(2)]
    cast_load_rows(k, wkv, k.w_in[:, C_KB:C_KB + 512], 16, 512, "wkv")
    xTv = k.xT.rearrange("(a p) c -> p a c", p=128)
    NG = S // 512

    def load(g):
        sl = g % 2
        for a in range(0, 16, 2):
            P.dma("pool", xc[sl][:, a:a + 2, :], xTv[:, a:a + 2, g * 512:(g + 1) * 512], writes=[("xc", sl, a)])
        P.dma("sp", rk[sl][:], k.ropeK[g * 512:(g + 1) * 512, :].rearrange("(j p) c -> p j c", p=128),
              writes=[("rk", sl)])

    def finish(tt):
        q = kn[tt % 2]
        qres = ("kn", tt % 2)
        pb = k.pb[tt % 2]
        for h in range(2):
            P.op("pe", lambda e, h=h: e.transpose(pb[:, h * 128:(h + 1) * 128], q[:, h * 128:(h + 1) * 128],
                                                  k.ident_bf[:]),
                 reads=[qres, "ident_bf"], writes=[("pb", tt % 2)])
        evac(k, "act", k.KbT[:, 0:2, tt * 128:(tt + 1) * 128],
             pb[:, 0:256].rearrange("p (h t) -> p h t", h=2), [("pb", tt % 2)], [("KbT", tt)])

    load(0)
    for g in range(NG):
        if g + 1 < NG:
            load(g + 1)
        sl = g % 2
        for j in range(4):
            tt = g * 4 + j
            b = tt % 4
            ps = k.pf[b]
            for a in range(16):
                P.op("pe", lambda e, a=a, j=j, ps=ps, sl=sl: e.matmul(
                    ps[:, :], xc[sl][:, a, j * 128:(j + 1) * 128], wkv[:, a, :], start=(a == 0), stop=(a == 15)),
                    reads=["wkv", ("xc", sl, a - a % 2)], writes=[("pf", b)])
            if tt >= 1:
                finish(tt - 1)
            evac(k, "act", k.Vb[:, tt, :], ps[:, 256:512], [("pf", b)], [("Vb", tt)])
            norm_rope(k, ps, 2, PP_GK, rk[sl][:, j, 0:128], rk[sl][:, j, 128:256], kn[tt % 2], ("pf", b),
                      ("kn", tt % 2), ("rk", sl))
    finish(S // 128 - 1)
    P.barrier()
    A.release("wkv", "xc0", "xc1", "rk0", "rk1", "kn0", "kn1")


def stage4(k):
    P, A = k.P, k.A
    pT = [A.alloc(f"pT{i}", [128, 512], BF16) for i in range(3)]
    rden = [A.alloc(f"rden{i}", [128, 512], F32) for i in range(2)]
    NKT = S // 128
    kres = [("KbT", t) for t in range(NKT)]
    vres = [("Vb", t) for t in range(NKT)]
    it = 0
    for qg in range(2):
        for h in range(NH):
            kvh = h // 4
            bo, bd = (2, 3) if it % 2 == 0 else (4, 5)
            accO, accD = k.pf[bo], k.pf[bd]
            qsl = slice(qg * 512, (qg + 1) * 512)

            def pv(kt, accO=accO, accD=accD, kvh=kvh, bo=bo, bd=bd):
                pt = pT[kt % 3]
                P.op("pe", lambda e, kt=kt, pt=pt, accO=accO, kvh=kvh: e.matmul(
                    accO[:, :], k.Vb[:, kt, kvh * 128:(kvh + 1) * 128], pt[:], start=(kt == 0), stop=(kt == NKT - 1)),
                    reads=[("pT", kt % 3), vres[kt]], writes=[("pf", bo)])
                P.op("pe", lambda e, kt=kt, pt=pt, accD=accD: e.matmul(accD[:, :], k.ones_bf[:], pt[:],
                                                                       start=(kt == 0), stop=(kt == NKT - 1)),
                     reads=[("pT", kt % 3), "ones_bf"], writes=[("pf", bd)])

            for kt in range(NKT):
                sb = kt % 2
                ST = k.pf[sb]
                P.op("pe", lambda e, kt=kt, ST=ST, kvh=kvh, h=h, qsl=qsl: e.matmul(
                    ST[:, :], k.KbT[:, kvh, kt * 128:(kt + 1) * 128], k.QbT[:, h, qsl], start=True, stop=True),
                     reads=[kres[kt], "QbT"], writes=[("pf", sb)])
                P.op("act", lambda e, kt=kt, ST=ST: e.activation(out=pT[kt % 3][:], in_=ST[:, :], func=AF.Exp,
                                                                 bias=k.cst[:, 5:6], scale=SCALE),
                     reads=[("pf", sb), "cst5"], writes=[("pT", kt % 3)])
                if kt >= 1:
                    pv(kt - 1)
            pv(NKT - 1)
            rd = rden[it % 2]
            P.op("dve", lambda e, rd=rd, accD=accD: e.reciprocal(out=rd[:], in_=accD[:, :]), reads=[("pf", bd)],
                 writes=[("rden", it % 2)])
            P.op("dve", lambda e, rd=rd, accO=accO, h=h, qsl=qsl: e.tensor_tensor(
                out=k.obT[:, h, qsl], in0=accO[:, :], in1=rd[:], op=ALU.mult),
                reads=[("pf", bo), ("rden", it % 2)], writes=["obT"])
            it += 1
    if k.dbg:
        P.dma("sp", k.dbgout["d_obT"][:], k.obT[:], reads=["obT"], writes=[("OUT", "d_obT")])
    P.barrier()
    A.release("pT0", "pT1", "pT2", "rden0", "rden1", "KbT", "Vb", "QbT")


def stage5(k):
    P, A = k.P, k.A
    k.mT = A.alloc("mT", [128, 16, TOK], BF16)
    xo = A.alloc("xo", [128, 16, TOK], BF16)
    wga = [A.alloc(f"wga{i}", [128, 16, 256], BF16) for i in range(2)]
    wgb = [A.alloc(f"wgb{i}", [128, 16, 256], BF16) for i in range(2)]
    wa = [A.alloc(f"wa{i}", [128, 8, 256], BF16) for i in range(2)]
    wb = [A.alloc(f"wb{i}", [128, 8, 256], BF16) for i in range(2)]
    sga = [A.alloc(f"sga{i}", [128, 512], F32) for i in range(2)]
    sgb = [A.alloc(f"sgb{i}", [128, 512], F32) for i in range(2)]
    tma = [A.alloc(f"tma{i}", [128, 512], F32) for i in range(2)]
    tmb = [A.alloc(f"tmb{i}", [128, 512], F32) for i in range(2)]
    xv = k.xTh.rearrange("(a p) c -> p a c", p=128)
    for a in range(16):
        P.dma("pool", xo[:, a, :], xv[:, a, HALO:HALO + TOK], writes=[("xo", a)])

    def load(cb):
        sl = cb % 2
        cast_load_rows(k, wga[sl], k.w_in[:, C_GA + cb * 256:C_GA + (cb + 1) * 256], 16, 256, ("wga", sl))
        cast_load_rows(k, wgb[sl], k.w_in[:, C_GB + cb * 256:C_GB + (cb + 1) * 256], 16, 256, ("wgb", sl))
        cast_load_rows(k, wa[sl], k.w_a[:, cb * 256:(cb + 1) * 256], 8, 256, ("wa", sl))
        cast_load_rows(k, wb[sl], k.w_b[:, cb * 256:(cb + 1) * 256], 8, 256, ("wb", sl))

    load(0)
    it = 0
    for cb in range(8):
        if cb + 1 < 8:
            load(cb + 1)
        sl = cb % 2
        for jj in range(2):
            j = cb * 2 + jj
            cs = slice(jj * 128, (jj + 1) * 128)
            for tg in range(2):
                ts_ = slice(tg * 512, (tg + 1) * 512)
                banks = [(4 * it + r) % 6 for r in range(4)]
                pa, pbr, pga, pgb = (k.pf[b] for b in banks)
                s2 = it % 2
                for h in range(NH):
                    P.op("pe", lambda e, h=h, pa=pa, sl=sl, cs=cs, ts_=ts_: e.matmul(
                        pa[:, :], wa[sl][:, h, cs], k.oaT[:, h, ts_], start=(h == 0), stop=(h == NH - 1)),
                        reads=[("wa", sl), "oaT"], writes=[("pf", banks[0])])
                for h in range(NH):
                    P.op("pe", lambda e, h=h, pbr=pbr, sl=sl, cs=cs, ts_=ts_: e.matmul(
                        pbr[:, :], wb[sl][:, h, cs], k.obT[:, h, ts_], start=(h == 0), stop=(h == NH - 1)),
                        reads=[("wb", sl), "obT"], writes=[("pf", banks[1])])
                for a in range(16):
                    P.op("pe", lambda e, a=a, pga=pga, sl=sl, cs=cs, ts_=ts_: e.matmul(
                        pga[:, :], wga[sl][:, a, cs], xo[:, a, ts_], start=(a == 0), stop=(a == 15)),
                        reads=[("wga", sl), ("xo", a)], writes=[("pf", banks[2])])
                for a in range(16):
                    P.op("pe", lambda e, a=a, pgb=pgb, sl=sl, cs=cs, ts_=ts_: e.matmul(
                        pgb[:, :], wgb[sl][:, a, cs], xo[:, a, ts_], start=(a == 0), stop=(a == 15)),
                        reads=[("wgb", sl), ("xo", a)], writes=[("pf", banks[3])])
                P.op("act", lambda e, pga=pga, s2=s2, j=j: e.activation(
                    out=sga[s2][:], in_=pga[:, :], func=AF.Sigmoid, bias=k.pp[:, PP_BG + j:PP_BG + j + 1], scale=1.0),
                    reads=[("pf", banks[2]), "pp"], writes=[("sga", s2)])
                P.op("act", lambda e, pgb=pgb, s2=s2, j=j: e.activation(
                    out=sgb[s2][:], in_=pgb[:, :], func=AF.Sigmoid, bias=k.pp[:, PP_BG + 16 + j:PP_BG + 17 + j],
                    scale=1.0), reads=[("pf", banks[3]), "pp"], writes=[("sgb", s2)])
                P.op("dve", lambda e, pa=pa, s2=s2: e.tensor_tensor(out=tma[s2][:], in0=pa[:, :], in1=sga[s2][:],
                                                                   op=ALU.mult),
                     reads=[("pf", banks[0]), ("sga", s2)], writes=[("tma", s2)])
                P.op("dve", lambda e, pbr=pbr, s2=s2: e.tensor_tensor(out=tmb[s2][:], in0=pbr[:, :], in1=sgb[s2][:],
                                                                     op=ALU.mult),
                     reads=[("pf", banks[1]), ("sgb", s2)], writes=[("tmb", s2)])
                P.op("dve", lambda e, s2=s2, j=j, ts_=ts_: e.tensor_tensor(out=k.mT[:, j, ts_], in0=tma[s2][:],
                                                                           in1=tmb[s2][:], op=ALU.add),
                     reads=[("tma", s2), ("tmb", s2)], writes=[("mT", j)])
                it += 1
    if k.dbg:
        P.dma("sp", k.dbgout["d_mT"][:], k.mT[:], reads=[("mT", j) for j in range(16)], writes=[("OUT", "d_mT")])
    P.barrier()
    A.release("xo", "wga0", "wga1", "wgb0", "wgb1", "wa0", "wa1", "wb0", "wb1", "sga0", "sga1", "sgb0", "sgb1",
              "tma0", "tma1", "tmb0", "tmb1", "oaT", "obT")
    A.release("nr_junk", "nr_ssq", "nr_t0", "nr_t1", "nr_t2")


def layer_norm_tile(k, xt, xres, lng, lnres, lnst, mv, tag):
    P = k.P
    P.op("dve", lambda e: e.bn_aggr(out=mv[:, 0:2], in_=lnst.rearrange("p a b -> p (a b)")),
         reads=[(tag, "st", d) for d in range(4)], writes=[(tag, "mv")])
    P.op("act", lambda e: e.activation(out=mv[:, 2:3], in_=mv[:, 1:2], func=AF.Sqrt, bias=k.cst[:, 2:3], scale=1.0),
         reads=[(tag, "mv"), "cst2"], writes=[(tag, "sd")])
    P.op("dve", lambda e: e.reciprocal(out=mv[:, 3:4], in_=mv[:, 2:3]), reads=[(tag, "sd")], writes=[(tag, "rs")])
    P.op("dve", lambda e: e.tensor_scalar(out=xt[:], in0=xt[:], scalar1=mv[:, 0:1], scalar2=mv[:, 3:4],
                                          op0=ALU.subtract, op1=ALU.mult), reads=[xres, (tag, "mv"), (tag, "rs")],
         writes=[xres])
    P.op("dve", lambda e: e.tensor_tensor(out=xt[:], in0=xt[:], in1=lng[:, 0, :], op=ALU.mult), reads=[xres, lnres],
         writes=[xres])
    P.op("dve", lambda e: e.tensor_tensor(out=xt[:], in0=xt[:], in1=lng[:, 1, :], op=ALU.add), reads=[xres, lnres],
         writes=[xres])


def stage6(k):
    P, A = k.P, k.A
    k.h1T = A.alloc("h1T", [128, 16, TOK], BF16)
    k.rtM = A.alloc("rtM", [128, NT, E], F32)
    k.rtW = A.alloc("rtW", [128, NT, E], F32)
    wo = A.alloc("wo", [128, 16, D], BF16)
    lng = A.alloc("lng", [128, 2, D], F32)
    xts = [A.alloc(f"xt{i}", [128, D], F32) for i in range(2)]
    racc = A.alloc("racc", [128, D], F32)
    h1bf = A.alloc("h1bf", [128, D], BF16)
    h1Tf = A.alloc("h1Tf", [128, 16, 128], F32)
    wr = A.alloc("wr", [128, 16, E], F32)
    lnst = A.alloc("lnst", [128, 4, 6], F32)
    mv = A.alloc("mv", [128, 8], F32)
    rs = A.alloc("rs", [128, 12, E], F32)
    r8 = A.alloc("r8", [128, 12, 8], F32)
    cmp3 = A.alloc("cmp3", [128, 8, 8], F32)
    ident_f = k.cp[:, CP_IDENT:CP_IDENT + 128]

    cast_load_rows(k, wo, k.w_out, 16, D, "wo")
    P.dma("sp", lng[:], k.lnp[:, 0:2, :], writes=["lng"])
    P.dma("sp", wr[:], k.w_router.rearrange("(a p) c -> p a c", p=128), writes=["wr"])
    P.op("dve", lambda e: e.memset(h1bf[:], 0.0), writes=["h1bf"])
    for z in range(CAP // 128):
        P.dma("sp", k.h1b[TOK + z * 128:TOK + (z + 1) * 128, :], h1bf[:], reads=["h1bf"], writes=[("H1BZ", z)])
    for i in range(NT):
        xt = xts[i % 2]
        xres = ("xt", i % 2)
        tsl = slice(i * 128, (i + 1) * 128)
        P.dma("sp", xt[:], k.x_own[tsl, :], writes=[xres])
        for dg in range(4):
            b = dg % 2
            ps = k.pf[b]
            dsl = slice(dg * 512, (dg + 1) * 512)
            for j in range(16):
                P.op("pe", lambda e, j=j, ps=ps, tsl=tsl, dsl=dsl: e.matmul(
                    ps[:, :], k.mT[:, j, tsl], wo[:, j, dsl], start=(j == 0), stop=(j == 15)),
                    reads=[("mT", j), "wo"], writes=[("pf", b)])
            P.op("dve", lambda e, ps=ps, xt=xt, dsl=dsl: e.scalar_tensor_tensor(
                out=xt[:, dsl], in0=xt[:, dsl], scalar=ALPHA, in1=ps[:, :], op0=ALU.mult, op1=ALU.add),
                reads=[xres, ("pf", b)], writes=[xres])
            P.op("dve", lambda e, xt=xt, dsl=dsl, dg=dg: e.bn_stats(out=lnst[:, dg, :], in_=xt[:, dsl]),
                 reads=[xres], writes=[("ln1", "st", dg)])
        layer_norm_tile(k, xt, xres, lng, "lng", lnst, mv, "ln1")
        P.op("act", lambda e, xt=xt: e.copy(out=h1bf[:], in_=xt[:]), reads=[xres], writes=["h1bf"])
        P.dma("sp", k.h1b[tsl, :], h1bf[:], reads=["h1bf"], writes=[("H1B", i)])
        P.op("act", lambda e, xt=xt: e.mul(out=racc[:], in_=xt[:], mul=ALPHA), reads=[xres], writes=["racc"])
        P.dma("sp", k.acc[tsl, :], racc[:], reads=["racc"], writes=[("ACCI", i)])
        if k.dbg:
            P.dma("sp", k.dbgout["d_h1"][tsl, :], xt[:], reads=[xres], writes=[("OUT", "d_h1", i)])
        for q4 in range(4):
            b = 2 + q4
            ps = k.pf[b]
            for r in range(4):
                a = q4 * 4 + r
                P.op("pe", lambda e, a=a, r=r, ps=ps, xt=xt: e.transpose(
                    ps[:, r * 128:(r + 1) * 128], xt[:, a * 128:(a + 1) * 128], ident_f),
                    reads=[xres, "cp"], writes=[("pf", b)])
            P.op("act", lambda e, q4=q4, ps=ps: e.copy(out=h1Tf[:, q4 * 4:(q4 + 1) * 4, :],
                                                       in_=ps[:, :].rearrange("p (r t) -> p r t", r=4)),
                 reads=[("pf", b)], writes=[("h1Tf", q4), ("pf", b)])
            P.op("dve", lambda e, q4=q4, ps=ps, tsl=tsl: e.tensor_copy(
                out=k.h1T[:, q4 * 4:(q4 + 1) * 4, tsl], in_=ps[:, :].rearrange("p (r t) -> p r t", r=4)),
                reads=[("pf", b)], writes=[("h1T", i)])
        lg = k.pf[0]
        for a in range(16):
            P.op("pe", lambda e, a=a: e.matmul(lg[:, 0:E], h1Tf[:, a, :], wr[:, a, :], start=(a == 0), stop=(a == 15)),
                 reads=[("h1Tf", a // 4), "wr"], writes=[("pf", 0)])
        sc, bz, eq, mk2, mvv, ws = (rs[:, r, :] for r in range(6))
        m1, m2, gs, cnt, gsel, pen, top8, wsum, rw = (r8[:, r, :] for r in range(9))
        v3 = lambda ap: ap.rearrange("p (g j) -> p g j", g=8)
        P.op("act", lambda e: e.activation(out=sc, in_=lg[:, 0:E], func=AF.Sigmoid), reads=[("pf", 0)], writes=["r_sc"])
        P.op("dve", lambda e: e.tensor_tensor(out=bz, in0=sc, in1=k.pp[:, PP_RB:PP_RB + E], op=ALU.add),
             reads=["r_sc", "pp"], writes=["r_bz"])
        P.op("dve", lambda e: e.reduce_max(out=m1, in_=v3(bz), axis=AX.X), reads=["r_bz"], writes=["r_m1"])
        P.op("dve", lambda e: e.tensor_tensor(out=v3(eq), in0=v3(bz), in1=m1.unsqueeze(2).to_broadcast([128, 8, 8]),
                                              op=ALU.is_equal), reads=["r_bz", "r_m1"], writes=["r_eq"])
        P.op("dve", lambda e: e.scalar_tensor_tensor(out=mk2, in0=eq, scalar=-BIG, in1=bz, op0=ALU.mult, op1=ALU.add),
             reads=["r_eq", "r_bz"], writes=["r_mk2"])
        P.op("dve", lambda e: e.reduce_max(out=m2, in_=v3(mk2), axis=AX.X), reads=["r_mk2"], writes=["r_m2"])
        P.op("dve", lambda e: e.tensor_tensor(out=gs, in0=m1, in1=m2, op=ALU.add), reads=["r_m1", "r_m2"],
             writes=["r_gs"])
        P.op("dve", lambda e: e.tensor_tensor(out=cmp3[:], in0=gs.unsqueeze(1).to_broadcast([128, 8, 8]),
                                              in1=gs.unsqueeze(2).to_broadcast([128, 8, 8]), op=ALU.is_gt),
             reads=["r_gs"], writes=["r_cmp"])
        P.op("dve", lambda e: e.reduce_sum(out=cnt, in_=cmp3[:], axis=AX.X), reads=["r_cmp"], writes=["r_cnt"])
        P.op("dve", lambda e: e.tensor_scalar(out=gsel, in0=cnt, scalar1=3.5, scalar2=None, op0=ALU.is_lt),
             reads=["r_cnt"], writes=["r_gsel"])
        P.op("dve", lambda e: e.tensor_scalar(out=pen, in0=gsel, scalar1=BIG, scalar2=-BIG, op0=ALU.mult, op1=ALU.add),
             reads=["r_gsel"], writes=["r_pen"])
        P.op("dve", lambda e: e.tensor_tensor(out=v3(mvv), in0=v3(bz), in1=gsel.unsqueeze(2).to_broadcast([128, 8, 8]),
                                              op=ALU.mult), reads=["r_bz", "r_gsel"], writes=["r_mv"])
        P.op("dve", lambda e: e.tensor_tensor(out=v3(mvv), in0=v3(mvv), in1=pen.unsqueeze(2).to_broadcast([128, 8, 8]),
                                              op=ALU.add), reads=["r_mv", "r_pen"], writes=["r_mv"])
        P.op("dve", lambda e: e.max(out=top8, in_=mvv), reads=["r_mv"], writes=["r_top8"])
        P.op("dve", lambda e, i=i: e.tensor_scalar(out=k.rtM[:, i, :], in0=mvv, scalar1=top8[:, 7:8], scalar2=None,
                                                   op0=ALU.is_ge), reads=["r_mv", "r_top8"], writes=[("rtM", i)])
        P.op("dve", lambda e, i=i: e.tensor_tensor(out=ws, in0=sc, in1=k.rtM[:, i, :], op=ALU.mult),
             reads=["r_sc", ("rtM", i)], writes=["r_ws"])
        P.op("dve", lambda e: e.reduce_sum(out=wsum[:, 0:1], in_=ws, axis=AX.X), reads=["r_ws"], writes=["r_wsum"])
        P.op("dve", lambda e: e.reciprocal(out=rw[:, 0:1], in_=wsum[:, 0:1]), reads=["r_wsum"], writes=["r_rw"])
        P.op("dve", lambda e, i=i: e.tensor_scalar(out=k.rtW[:, i, :], in0=ws, scalar1=rw[:, 0:1], scalar2=2.5,
                                                   op0=ALU.mult, op1=ALU.mult), reads=["r_ws", "r_rw"],
             writes=[("rtW", i)])
    P.barrier()
    A.release("wo", "lng", "xt0", "xt1", "racc", "h1bf", "h1Tf", "wr", "lnst", "mv", "rs", "r8", "cmp3", "mT")


def stage7(k):
    P, A = k.P, k.A
    Mbf = A.alloc("Mbf", [128, NT, E], BF16)
    rankp = A.alloc("rankp", [128, NT, E], F32)
    VW = A.alloc("VW", [128, NT, E, 6], BF16)
    wr1 = A.alloc("wr1", [128, E], F32)
    wr2 = A.alloc("wr2", [128, E], F32)
    k.idx = A.alloc("idx", [128, E, 2], I32)
    k.wsl = A.alloc("wsl", [128, E, 2], F32)
    Sb = [A.alloc(f"Sb{i}", [128, CAP], BF16) for i in range(2 * NT)]
    padv = A.alloc("padv", [128, 2], F32)
    k.hsT = A.alloc("hsT", [128, 4, TOK], BF16)
    wsg = A.alloc("wsg", [128, 16, FF], BF16)
    wsu = A.alloc("wsu", [128, 16, FF], BF16)
    sgs = [A.alloc(f"sgs{i}", [128, 512], F32) for i in range(2)]
    cast_load_rows(k, wsg, k.w_sg, 16, FF, "wsg")
    cast_load_rows(k, wsu, k.w_su, 16, FF, "wsu")
    iota = k.cp[:, CP_IOTA:CP_IOTA + CAP]
    for i in range(NT):
        P.op("dve", lambda e, i=i: e.tensor_copy(out=Mbf[:, i, :], in_=k.rtM[:, i, :]), reads=[("rtM", i)],
             writes=[("Mbf", i)])
        P.op("dve", lambda e, i=i: e.tensor_copy(out=VW[:, i, :, 0],
                                                 in_=k.cp[:, CP_VCOL + i:CP_VCOL + i + 1].to_broadcast([128, E])),
             reads=["cp2"], writes=[("VW0", i)])
        P.op("dve", lambda e, i=i: e.tensor_copy(out=VW[:, i, :, 1],
                                                 in_=k.cp[:, CP_VCOL + 8 + i:CP_VCOL + 9 + i].to_broadcast([128, E])),
             reads=["cp2"], writes=[("VW0b", i)])
        P.op("dve", lambda e, i=i: e.tensor_copy(out=VW[:, i, :, 2], in_=k.rtW[:, i, :]), reads=[("rtW", i)],
             writes=[("VW1", i)])
        P.op("dve", lambda e, i=i: e.tensor_tensor(out=wr1[:], in0=k.rtW[:, i, :], in1=VW[:, i, :, 2], op=ALU.subtract),
             reads=[("rtW", i), ("VW1", i)], writes=["wr1"])
        P.op("dve", lambda e, i=i: e.tensor_copy(out=VW[:, i, :, 3], in_=wr1[:]), reads=["wr1"], writes=[("VW1b", i)])
        P.op("dve", lambda e, i=i: e.tensor_tensor(out=wr2[:], in0=wr1[:], in1=VW[:, i, :, 3], op=ALU.subtract),
             reads=["wr1", ("VW1b", i)], writes=["wr2"])
        P.op("dve", lambda e, i=i: e.tensor_copy(out=VW[:, i, :, 4], in_=wr2[:]), reads=["wr2"], writes=[("VW1c", i)])
        P.op("dve", lambda e, i=i: e.memset(VW[:, i, :, 5], 1.0), writes=[("VW2", i)])
    for j in range(NT):
        ps = k.pf[4]
        for i in range(j):
            P.op("pe", lambda e, i=i, j=j: e.matmul(ps[:, 0:E], k.ones_bf[:], Mbf[:, i, :], start=(i == 0), stop=False),
                 reads=["ones_bf", ("Mbf", i)], writes=[("pf", 4)])
        P.op("pe", lambda e, j=j: e.matmul(ps[:, 0:E], k.u_bf[:], Mbf[:, j, :], start=(j == 0), stop=True),
             reads=["u_bf", ("Mbf", j)], writes=[("pf", 4)])
        P.op("dve", lambda e, j=j: e.scalar_tensor_tensor(out=rankp[:, j, :], in0=ps[:, 0:E], scalar=1.0,
                                                          in1=k.rtM[:, j, :], op0=ALU.add, op1=ALU.mult),
             reads=[("pf", 4), ("rtM", j)], writes=[("rankp", j)])
        P.op("dve", lambda e, j=j: e.tensor_scalar(out=rankp[:, j, :], in0=rankp[:, j, :], scalar1=-1.0, scalar2=None,
                                                   op0=ALU.add), reads=[("rankp", j)], writes=[("rankp", j)])
    h1res = [("h1T", i) for i in range(NT)]

    def shared_hidden(ft, tg, it):
        bg, bu = (0, 1) if it % 2 == 0 else (2, 3)
        G, U = k.pf[bg], k.pf[bu]
        ts_ = slice(tg * 512, (tg + 1) * 512)
        fs = slice(ft * 128, (ft + 1) * 128)
        for a in range(16):
            P.op("pe", lambda e, a=a: e.matmul(G[:, :], wsg[:, a, fs], k.h1T[:, a, ts_], start=(a == 0), stop=(a == 15)),
                 reads=["wsg"] + h1res, writes=[("pf", bg)])
        for a in range(16):
            P.op("pe", lambda e, a=a: e.matmul(U[:, :], wsu[:, a, fs], k.h1T[:, a, ts_], start=(a == 0), stop=(a == 15)),
                 reads=["wsu"] + h1res, writes=[("pf", bu)])
        sg = sgs[it % 2]
        P.op("act", lambda e: e.activation(out=sg[:], in_=G[:, :], func=AF.Silu), reads=[("pf", bg)],
             writes=[("sgs", it % 2)])
        P.op("dve", lambda e: e.tensor_tensor(out=k.hsT[:, ft, ts_], in0=sg[:], in1=U[:, :], op=ALU.mult),
             reads=[("sgs", it % 2), ("pf", bu)], writes=["hsT"])

    sh_list = [(ft, tg) for ft in range(4) for tg in range(2)]
    sh_i = 0
    tw = k.pf[5]
    for ex in range(E):
        for i in range(NT):
            Sx = Sb[(ex % 2) * NT + i]
            P.op("dve", lambda e, Sx=Sx, i=i, ex=ex: e.tensor_scalar(out=Sx[:], in0=iota,
                                                                      scalar1=rankp[:, i, ex:ex + 1], scalar2=None,
                                                                      op0=ALU.is_equal),
                 reads=["cp", ("rankp", i)], writes=[("Sb", (ex % 2) * NT + i)])
        tw = k.pf[4 + ex % 2]
        twres = ("pf", 4 + ex % 2)
        cb = (ex // 2) * 12
        for sb in range(2):
            c0 = cb + sb * 6
            for i in range(NT):
                Sx = Sb[(ex % 2) * NT + i]
                P.op("pe", lambda e, Sx=Sx, i=i, ex=ex, sb=sb, c0=c0, tw=tw: e.matmul(
                    tw[:, c0:c0 + 6], Sx[:, sb * 128:(sb + 1) * 128], VW[:, i, ex, :], start=(i == 0),
                    stop=(i == NT - 1)),
                    reads=[("Sb", (ex % 2) * NT + i), ("VW0", i), ("VW0b", i), ("VW1", i), ("VW1b", i), ("VW1c", i),
                           ("VW2", i)], writes=[twres])
        twv = tw[:, cb:cb + 12].rearrange("p (s c) -> p s c", c=6)
        P.op("dve", lambda e, twv=twv: e.tensor_scalar(out=padv[:], in0=twv[:, :, 5], scalar1=-1.0, scalar2=1.0,
                                                       op0=ALU.mult, op1=ALU.add), reads=[twres], writes=["padv"])
        P.op("dve", lambda e: e.tensor_tensor(out=padv[:], in0=padv[:], in1=k.cp[:, CP_SLOT:CP_SLOT + 2], op=ALU.mult),
             reads=["padv", "cp2"], writes=["padv"])
        P.op("dve", lambda e, twv=twv: e.scalar_tensor_tensor(out=padv[:], in0=twv[:, :, 0], scalar=32.0,
                                                              in1=padv[:], op0=ALU.mult, op1=ALU.add),
             reads=[twres, "padv"], writes=["padv"])
        P.op("dve", lambda e, twv=twv: e.scalar_tensor_tensor(out=padv[:], in0=twv[:, :, 1], scalar=float(TOK),
                                                              in1=padv[:], op0=ALU.add, op1=ALU.add),
             reads=[twres, "padv"], writes=["padv"])
        P.op("dve", lambda e, ex=ex: e.tensor_copy(out=k.idx[:, ex, :], in_=padv[:]), reads=["padv"],
             writes=[("idx", ex)])
        P.op("act", lambda e, ex=ex, twv=twv: e.copy(out=k.wsl[:, ex, :], in_=twv[:, :, 2]), reads=[twres],
             writes=[("wsl", ex)])
        P.op("dve", lambda e, ex=ex, twv=twv: e.tensor_tensor(out=k.wsl[:, ex, :], in0=k.wsl[:, ex, :], in1=twv[:, :, 3],
                                                              op=ALU.add), reads=[twres, ("wsl", ex)],
             writes=[("wsl", ex)])
        P.op("dve", lambda e, ex=ex, twv=twv: e.tensor_tensor(out=k.wsl[:, ex, :], in0=k.wsl[:, ex, :], in1=twv[:, :, 4],
                                                              op=ALU.add), reads=[twres, ("wsl", ex)],
             writes=[("wsl", ex)])
        if ex % 8 == 7 and sh_i < len(sh_list):
            shared_hidden(*sh_list[sh_i], sh_i)
            sh_i += 1
    while sh_i < len(sh_list):
        shared_hidden(*sh_list[sh_i], sh_i)
        sh_i += 1
    if k.dbg:
        P.dma("sp", k.dbgout["d_rt"][:, :, 0, :], k.rtM[:], reads=[("rtM", i) for i in range(NT)],
              writes=[("OUT", "rt0")])
        P.dma("sp", k.dbgout["d_rt"][:, :, 1, :], k.rtW[:], reads=[("rtW", i) for i in range(NT)],
              writes=[("OUT", "rt1")])
        P.dma("sp", k.dbgout["d_rt"][:, :, 2, :], rankp[:], reads=[("rankp", i) for i in range(NT)],
              writes=[("OUT", "rt2")])
        P.dma("sp", k.dbgout["d_idx"][:], k.idx[:], reads=[("idx", ex) for ex in range(E)], writes=[("OUT", "idx")])
    P.barrier()
    A.release("padv", "wr1", "wr2", "Mbf", "rankp", "VW", *[f"Sb{i}" for i in range(2 * NT)], "wsg", "wsu", "sgs0", "sgs1", "h1T", "rtM", "rtW")


def stage7b(k):
    P, A = k.P, k.A
    EL = getattr(k, "EL", E)
    wg = [A.alloc(f"wg{i}", [128, 16, FF], BF16) for i in range(2)]
    wu = [A.alloc(f"wu{i}", [128, 16, FF], BF16) for i in range(2)]
    wd = [A.alloc(f"wd{i}", [128, 4, D], BF16) for i in range(2)]
    xe = [A.alloc(f"xe{i}", [128, 2, D], BF16) for i in range(2)]
    xeT = [A.alloc(f"xeT{i}", [128, 16, CAP], BF16) for i in range(2)]
    hid = [A.alloc(f"hid{i}", [128, 4, CAP], BF16) for i in range(2)]
    sgt = [A.alloc(f"sgt{i}", [128, CAP], F32) for i in range(2)]
    yst = [A.alloc(f"yst{i}", [128, D], F32) for i in range(4)]
    h1bres = [("H1B", i) for i in range(NT)] + [("H1BZ", z) for z in range(CAP // 128)]
    accires = [("ACCI", i) for i in range(NT)]

    def load_w(ex):
        sl = ex % 2
        cast_load_rows(k, wg[sl], k.w_eg[ex], 16, FF, ("wg", sl))
        cast_load_rows(k, wu[sl], k.w_eu[ex], 16, FF, ("wu", sl))
        cast_load_rows(k, wd[sl], k.w_ed[ex], 4, D, ("wd", sl))

    def gather(ex):
        sl = ex % 2
        for sb in range(2):
            P.op("pool", lambda e, ex=ex, sb=sb, sl=sl: e.indirect_dma_start(
                out=xe[sl][:, sb, :], out_offset=None, in_=k.h1b[:, :],
                in_offset=bass.IndirectOffsetOnAxis(ap=k.idx[:, ex, sb:sb + 1], axis=0)), reads=[("idx", ex)] + h1bres, writes=[("xe", sl, sb)], dma=True)

    load_w(0)
    gather(0)
    evn = 0
    MODE = getattr(k, "S7MODE", 3)
    for ex in range(EL):
        if ex + 1 < EL:
            load_w(ex + 1)
            gather(ex + 1)
        sl = ex % 2
        if MODE == 1:
            P.op("dve", lambda e: e.memset(k.cst[:, 6:7], 0.0), reads=[("wg", sl), ("wu", sl), ("wd", sl), ("xe", sl, 0),
                                                                      ("xe", sl, 1)], writes=["cst6"])
            continue
        for sb in range(2):
            for hf in range(2):
                pbi = (sb * 2 + hf) % 2
                pbk = k.pb[pbi]
                for a8 in range(8):
                    a = hf * 8 + a8
                    P.op("pe", lambda e, a=a, a8=a8, sb=sb, sl=sl, pbk=pbk: e.transpose(
                        pbk[:, a8 * 128:(a8 + 1) * 128], xe[sl][:, sb, a * 128:(a + 1) * 128], k.ident_bf[:]),
                        reads=[("xe", sl, sb), "ident_bf"], writes=[("pb", pbi)])
                evac(k, "act", xeT[sl][:, hf * 8:(hf + 1) * 8, sb * 128:(sb + 1) * 128],
                     pbk[:, :].rearrange("p (a t) -> p a t", a=8), [("pb", pbi)], [("xeT", sl, sb, hf)])
        xres = [("xeT", sl, sb, hf) for sb in range(2) for hf in range(2)]
        if MODE == 4:
            P.op("dve", lambda e: e.memset(k.cst[:, 6:7], 0.0), reads=xres + [("wg", sl), ("wu", sl), ("wd", sl)],
                 writes=["cst6"])
            continue
        for ft in range(4):
            bg, bu = (0, 1) if ft % 2 == 0 else (2, 3)
            G, U = k.pf[bg], k.pf[bu]
            fs = slice(ft * 128, (ft + 1) * 128)
            for a in range(16):
                P.op("pe", lambda e, a=a, G=G, fs=fs, sl=sl: e.matmul(
                    G[:, 0:CAP], wg[sl][:, a, fs], xeT[sl][:, a, :], start=(a == 0), stop=(a == 15)),
                    reads=[("wg", sl)] + xres, writes=[("pf", bg)])
            for a in range(16):
                P.op("pe", lambda e, a=a, U=U, fs=fs, sl=sl: e.matmul(
                    U[:, 0:CAP], wu[sl][:, a, fs], xeT[sl][:, a, :], start=(a == 0), stop=(a == 15)),
                    reads=[("wu", sl)] + xres, writes=[("pf", bu)])
            sg = sgt[ft % 2]
            P.op("act", lambda e, G=G, sg=sg: e.activation(out=sg[:], in_=G[:, 0:CAP], func=AF.Silu),
                 reads=[("pf", bg)], writes=[("sgt", ft % 2)])
            P.op("dve", lambda e, U=U, sg=sg, ft=ft, sl=sl: e.tensor_tensor(
                out=hid[sl][:, ft, :], in0=sg[:], in1=U[:, 0:CAP], op=ALU.mult),
                reads=[("sgt", ft % 2), ("pf", bu)], writes=[("hid", sl, ft)])
        hres = [("hid", sl, ft) for ft in range(4)]
        if MODE == 5:
            P.op("dve", lambda e: e.memset(k.cst[:, 6:7], 0.0), reads=hres + [("wd", sl)], writes=["cst6"])
            continue
        for sb in range(2):
            yb = (ex % 2) * 2 + sb
            y = yst[yb]
            for dg in range(4):
                b = 4 + evn % 2
                evn += 1
                bank = k.pf[b]
                dsl = slice(dg * 512, (dg + 1) * 512)
                for ft in range(4):
                    P.op("pe", lambda e, ft=ft, bank=bank, sb=sb, sl=sl, dsl=dsl: e.matmul(
                        bank[:, :], hid[sl][:, ft, sb * 128:(sb + 1) * 128], wd[sl][:, ft, dsl], start=(ft == 0),
                        stop=(ft == 3)), reads=hres + [("wd", sl)], writes=[("pf", b)])
                P.op("dve", lambda e, bank=bank, y=y, dsl=dsl, ex=ex, sb=sb: e.tensor_scalar(
                    out=y[:, dsl], in0=bank[:, :], scalar1=k.wsl[:, ex, sb:sb + 1], scalar2=None, op0=ALU.mult),
                    reads=[("pf", b), ("wsl", ex)], writes=[("yst", yb)])
            if MODE == 2:
                continue
            P.op("pool", lambda e, ex=ex, sb=sb, y=y: e.indirect_dma_start(
                out=k.acc[:, :], out_offset=bass.IndirectOffsetOnAxis(ap=k.idx[:, ex, sb:sb + 1], axis=0),
                in_=y[:, :], in_offset=None, compute_op=ALU.add),
                reads=[("yst", yb), ("idx", ex)] + accires + ([("ACCW", ex - 1, 0), ("ACCW", ex - 1, 1)] if ex else []),
                writes=[("ACCW", ex, sb)], dma=True)
    P.op("dve", lambda e: e.memset(k.cst[:, 6:7], 0.0), reads=[("ACCW", EL - 1, 0), ("ACCW", EL - 1, 1)],
         writes=["ACC"])
    P.barrier()
    A.release("wg0", "wg1", "wu0", "wu1", "wd0", "wd1", "xe0", "xe1", "xeT0", "xeT1", "hid0", "hid1", "sgt0", "sgt1",
              "yst0", "yst1", "yst2", "yst3", "idx", "wsl")


def stage8(k):
    P, A = k.P, k.A
    wsd = A.alloc("wsd", [128, 4, D], BF16)
    lng = A.alloc("lng2", [128, 2, D], F32)
    ats = [A.alloc(f"at{i}", [128, D], F32) for i in range(2)]
    lnst = A.alloc("lnst2", [128, 4, 6], F32)
    mv = A.alloc("mv2", [128, 8], F32)
    cast_load_rows(k, wsd, k.w_sd, 4, D, "wsd")
    P.dma("sp", lng[:], k.lnp[:, 2:4, :], writes=["lng2"])
    for i in range(NT):
        at = ats[i % 2]
        ares = ("at", i % 2)
        tsl = slice(i * 128, (i + 1) * 128)
        P.dma("sp", at[:], k.acc[tsl, :], reads=["ACC", ("ACCI", i)], writes=[ares])
        for dg in range(4):
            b = dg % 2
            ps = k.pf[b]
            dsl = slice(dg * 512, (dg + 1) * 512)
            for ft in range(4):
                P.op("pe", lambda e, ft=ft, ps=ps, tsl=tsl, dsl=dsl: e.matmul(
                    ps[:, :], k.hsT[:, ft, tsl], wsd[:, ft, dsl], start=(ft == 0), stop=(ft == 3)),
                    reads=["hsT", "wsd"], writes=[("pf", b)])
            P.op("dve", lambda e, ps=ps, at=at, dsl=dsl: e.tensor_tensor(out=at[:, dsl], in0=at[:, dsl], in1=ps[:, :],
                                                                        op=ALU.add), reads=[ares, ("pf", b)],
                 writes=[ares])
            P.op("dve", lambda e, at=at, dsl=dsl, dg=dg: e.bn_stats(out=lnst[:, dg, :], in_=at[:, dsl]), reads=[ares],
                 writes=[("ln2", "st", dg)])
        layer_norm_tile(k, at, ares, lng, "lng2", lnst, mv, "ln2")
        P.dma("sp", k.out[tsl, :], at[:], reads=[ares], writes=[("OUT", "out", i)])


def _t5_bucket_np():
    import jax
    import jax.numpy as jnp
    with jax.default_device(jax.devices("cpu")[0]):
        qi = jnp.arange(128)[:, None]
        c = jnp.arange(384)[None, :]
        rel = c - 128 - qi
        nb = 16
        ret = jnp.where(rel > 0, nb, 0)
        n = jnp.abs(rel)
        max_exact = nb // 2
        nf = jnp.maximum(n, 1).astype(jnp.float32)
        large = max_exact + (jnp.log(nf / max_exact) / math.log(128 / max_exact) * (nb - max_exact)).astype(jnp.int32)
        large = jnp.minimum(large, nb - 1)
        bucket = np.asarray(ret + jnp.where(n < max_exact, n, large))
        rel = np.asarray(rel)
    return bucket, rel


def _rope_tables():
    import jax
    import jax.numpy as jnp
    with jax.default_device(jax.devices("cpu")[0]):
        rows = S // 64
        row = jnp.broadcast_to(jnp.arange(rows)[:, None], (rows, 64)).reshape(S).astype(jnp.float32)
        col = jnp.broadcast_to(jnp.arange(64)[None, :], (rows, 64)).reshape(S).astype(jnp.float32)
        half = DH // 2
        inv = 10000.0 ** (-jnp.arange(0, half, 2, dtype=jnp.float32) / half)
        ang_r = row[:, None] * inv
        ang_c = col[:, None] * inv
        cr, sr, cc, sc = (np.asarray(t) for t in (jnp.cos(ang_r), jnp.sin(ang_r), jnp.cos(ang_c), jnp.sin(ang_c)))
    Ct = np.concatenate([cr, cr, cc, cc], axis=1)
    St = np.concatenate([-sr, sr, -sc, sc], axis=1)
    return np.ascontiguousarray(np.concatenate([Ct, St], axis=1).astype(np.float32))


def _const_pack():
    cp = np.zeros((128, CP_N), np.float32)
    cp[:, CP_IDENT:CP_IDENT + 128] = np.eye(128, dtype=np.float32)
    cp[:, CP_IOTA:CP_IOTA + 256] = np.arange(256, dtype=np.float32)[None, :]
    cp[:, CP_U:CP_U + 128] = np.triu(np.ones((128, 128), np.float32), 1)
    bucket, rel = _t5_bucket_np()
    valid = np.abs(rel) <= 128
    cp[:, CP_IDXA:CP_IDXA + 384] = np.where(valid, bucket, -1).astype(np.float32)
    cp[:, CP_MASKA:CP_MASKA + 384] = np.where(valid, 0.0, NEG).astype(np.float32)
    for i in range(NT):
        v = np.arange(128) + i * 128 - TOK
        cp[:, CP_VCOL + i] = np.floor_divide(v, 32).astype(np.float32)
        cp[:, CP_VCOL + 8 + i] = (v - 32 * np.floor_divide(v, 32)).astype(np.float32)
    cp[:, CP_SLOT] = np.arange(128, dtype=np.float32)
    cp[:, CP_SLOT + 1] = np.arange(128, dtype=np.float32) + 128
    return cp


def make_in_maps(inputs):
    f = lambda a: np.ascontiguousarray(np.asarray(a, dtype=np.float32))
    x = f(inputs["x"]).reshape(S, D)
    xT = np.ascontiguousarray(x.T)
    xTpad = np.zeros((D, S + 2 * HALO), np.float32)
    xTpad[:, HALO:HALO + S] = xT
    ropeK = _rope_tables()
    cp = _const_pack()
    shared = dict(
        xT=xT, w_in=f(inputs["w_in"]).reshape(D, IN_COLS), w_branch_a=f(inputs["w_branch_a"]).reshape(NH * DH, D),
        w_branch_b=f(inputs["w_branch_b"]).reshape(NH * DH, D), w_out=f(inputs["w_out"]).reshape(D, D),
        w_router=f(inputs["w_router"]).reshape(D, E), w_exp_gate=f(inputs["w_exp_gate"]).reshape(E, D, FF),
        w_exp_up=f(inputs["w_exp_up"]).reshape(E, D, FF), w_exp_down=f(inputs["w_exp_down"]).reshape(E, FF, D),
        w_sh_gate=f(inputs["w_sh_gate"]).reshape(D, FF), w_sh_up=f(inputs["w_sh_up"]).reshape(D, FF),
        w_sh_down=f(inputs["w_sh_down"]).reshape(FF, D), cpack=cp, ropeK=ropeK)
    lnp = np.stack([f(inputs[n]).reshape(D) for n in ("ln1_g", "ln1_b", "ln2_g", "ln2_b")])
    shared["lnp"] = np.ascontiguousarray(np.broadcast_to(lnp[None], (128, 4, D)))
    pp = np.zeros((128, PP_N), np.float32)
    pp[:, PP_SINK:PP_SINK + 8] = f(inputs["attn_sink"]).reshape(1, 8)
    pp[:, PP_TAB:PP_TAB + 256] = f(inputs["rel_bias_table"]).reshape(1, 256)
    pp[:, PP_GQ:PP_GQ + 128] = f(inputs["q_norm_g"]).reshape(1, 128)
    pp[:, PP_GK:PP_GK + 128] = f(inputs["k_norm_g"]).reshape(1, 128)
    pp[:, PP_RB:PP_RB + 64] = f(inputs["router_bias"]).reshape(1, 64)
    pp[:, PP_BG:PP_BG + 32] = f(inputs["b_gate"]).reshape(2, 16, 128).transpose(2, 0, 1).reshape(128, 32)
    maps = []
    for c in range(NCORES):
        m = dict(shared)
        m["xTh"] = np.ascontiguousarray(xTpad[:, c * TOK:c * TOK + TH])
        m["x_own"] = np.ascontiguousarray(x[c * TOK:(c + 1) * TOK])
        m["ropeQ"] = np.ascontiguousarray(ropeK[c * TOK:(c + 1) * TOK])
        ppc = pp.copy()
        ppc[:, PP_EDGE] = NEG if c == 0 else 0.0
        ppc[:, PP_EDGE + 1] = NEG if c == NCORES - 1 else 0.0
        m["ppack"] = ppc
        maps.append(m)
    return maps


_CACHE = {}


def kernel(**inputs):
    if "nc" not in _CACHE:
        _CACHE["nc"] = build_program()[0]
    nc = _CACHE["nc"]
    maps = make_in_maps(inputs)
    res = run_bass_kernel_spmd(nc, maps, core_ids=list(range(NCORES)))
    out = np.concatenate([np.asarray(r["out"], dtype=np.float32) for r in res.results], axis=0)
    return out.reshape(1, S, D)
```
